# Optimizing a Trainium2 kernel written in Bass

```python
import math
import jax, jax.numpy as jnp
from jax import lax

D_MODEL = 1024
BATCH = 4
SEQ = 4096
DEPTH = 2

RET_HEADS = 4
RET_DK = 128
RET_DV = 128
RET_CHUNK = 128
ROPE_BASE = 10000.0
S5_WIDTH = D_MODEL // 2
S5_GROUP = 16
S5_GROUPS = S5_WIDTH // S5_GROUP
S5_STATE = 64
S5_DT_MIN = 1e-3
S5_DT_MAX = 1e-1
ATT_HEADS = 8
ATT_DH = D_MODEL // ATT_HEADS
DILATED_BRANCHES = ((128, 1), (512, 4), (2048, 16))
N_GROUPS = 4
EXPERTS_PER_GROUP = 4
TOP_K = 2
D_EXPERT = 512
RMS_EPS = 1e-6
GN_EPS = 1e-6
N_EVEN = (DEPTH + 1) // 2
N_ODD = DEPTH // 2
RET_QK_W = RET_HEADS * RET_DK
RET_V_W = RET_HEADS * RET_DV
AB_IN_W = 2 * RET_QK_W + 2 * RET_V_W + S5_WIDTH
AB_OUT_W = RET_V_W + S5_WIDTH

kernel_name = "hybrid_retention_s5_dilated_hmoe"


def rmsnorm(x, g):
    xf = x.astype(jnp.float32)
    y = xf * lax.rsqrt(jnp.mean(xf * xf, axis=-1, keepdims=True) + RMS_EPS)
    return (y * g.astype(jnp.float32)).astype(x.dtype)


def rotary(x, pos):
    half = x.shape[-1] // 2
    inv = ROPE_BASE ** (-jnp.arange(half, dtype=jnp.float32) / half)
    ang = pos.astype(jnp.float32)[:, None] * inv[None, :]
    cos = jnp.cos(ang)[None, :, None, :]
    sin = jnp.sin(ang)[None, :, None, :]
    xf = x.astype(jnp.float32)
    x1, x2 = xf[..., :half], xf[..., half:]
    return jnp.concatenate([x1 * cos - x2 * sin, x1 * sin + x2 * cos], axis=-1)


def retention(q, k, v):
    B, L, H, _ = q.shape
    C = RET_CHUNK
    N = L // C
    lg = jnp.log1p(-jnp.exp2(-5.0 - jnp.arange(H, dtype=jnp.float32)))
    idx = jnp.arange(C, dtype=jnp.float32)
    diff = idx[:, None] - idx[None, :]
    decay = jnp.where(diff[None] >= 0, jnp.exp(jnp.maximum(diff, 0.0)[None] * lg[:, None, None]), 0.0)
    xi = jnp.exp((idx + 1.0)[None, :] * lg[:, None])
    zeta = jnp.exp((C - 1.0 - idx)[None, :] * lg[:, None])
    chunk_decay = jnp.exp(C * lg)

    def to_chunks(t):
        return t.reshape(B, N, C, H, t.shape[-1]).transpose(0, 3, 1, 2, 4)

    qc, kc, vc = to_chunks(q), to_chunks(k), to_chunks(v)
    scores = jnp.einsum('bhncd,bhnsd->bhncs', qc, kc) * decay[None, :, None]
    intra = jnp.einsum('bhncs,bhnse->bhnce', scores, vc)
    kv = jnp.einsum('bhnsd,bhnse->bhnde', kc * zeta[None, :, None, :, None], vc)

    def step(state, kv_n):
        return state * chunk_decay[None, :, None, None] + kv_n, state

    init = jnp.zeros((B, H, kv.shape[-2], kv.shape[-1]), jnp.float32)
    _, prev = lax.scan(step, init, jnp.moveaxis(kv, 2, 0))
    prev = jnp.moveaxis(prev, 0, 2)
    cross = jnp.einsum('bhncd,bhnde->bhnce', qc * xi[None, :, None, :, None], prev)
    return (intra + cross).transpose(0, 2, 3, 1, 4).reshape(B, L, H, -1)


def s5_mixer(u, a_re, a_im, b_re, b_im, c_re, c_im, d_skip, log_dt, w_glu, b_glu):
    B, L, _ = u.shape
    f32 = jnp.float32
    uf = u.astype(f32).reshape(B, L, S5_GROUPS, S5_GROUP)
    a_re, a_im = a_re.astype(f32), a_im.astype(f32)
    b_re, b_im = b_re.astype(f32), b_im.astype(f32)
    dt = jnp.exp(log_dt.astype(f32))[:, None]
    zr, zi = a_re * dt, a_im * dt
    mag = jnp.exp(zr)
    lam_re, lam_im = mag * jnp.cos(zi), mag * jnp.sin(zi)
    nr, ni = lam_re - 1.0, lam_im
    den = a_re * a_re + a_im * a_im
    coef_re = (nr * a_re + ni * a_im) / den
    coef_im = (ni * a_re - nr * a_im) / den
    bb_re = coef_re[..., None] * b_re - coef_im[..., None] * b_im
    bb_im = coef_re[..., None] * b_im + coef_im[..., None] * b_re
    bu_re = jnp.einsum('blgh,gph->blgp', uf, bb_re)
    bu_im = jnp.einsum('blgh,gph->blgp', uf, bb_im)
    ar = jnp.broadcast_to(lam_re, bu_re.shape)
    ai = jnp.broadcast_to(lam_im, bu_im.shape)

    def combine(e1, e2):
        a1r, a1i, b1r, b1i = e1
        a2r, a2i, b2r, b2i = e2
        return (a1r * a2r - a1i * a2i,
                a1r * a2i + a1i * a2r,
                a2r * b1r - a2i * b1i + b2r,
                a2r * b1i + a2i * b1r + b2i)

    _, _, xr, xim = lax.associative_scan(combine, (ar, ai, bu_re, bu_im), axis=1)
    y = (jnp.einsum('blgp,ghp->blgh', xr, c_re.astype(f32))
         - jnp.einsum('blgp,ghp->blgh', xim, c_im.astype(f32))
         + d_skip.astype(f32) * uf)
    g = jax.nn.gelu(y.reshape(B, L, S5_WIDTH))
    return g * jax.nn.sigmoid(g @ w_glu.astype(f32) + b_glu.astype(f32))


def parallel_retention_s5(xn, w_in, w_out, a_re, a_im, b_re, b_im, c_re, c_im, d_skip, log_dt, w_glu, b_glu, pos):
    B, L, _ = xn.shape
    proj = xn @ w_in
    q, k, v, gate, u = jnp.split(
        proj, [RET_QK_W, 2 * RET_QK_W, 2 * RET_QK_W + RET_V_W, 2 * RET_QK_W + 2 * RET_V_W], axis=-1)
    q = rotary(q.reshape(B, L, RET_HEADS, RET_DK), pos)
    k = rotary(k.reshape(B, L, RET_HEADS, RET_DK), pos) * (RET_DK ** -0.5)
    v = v.astype(jnp.float32).reshape(B, L, RET_HEADS, RET_DV)
    o = retention(q, k, v)
    mu = jnp.mean(o, axis=-1, keepdims=True)
    var = jnp.mean(jnp.square(o - mu), axis=-1, keepdims=True)
    o = (o - mu) * lax.rsqrt(var + GN_EPS)
    ret_out = jax.nn.silu(gate.astype(jnp.float32)) * o.reshape(B, L, RET_V_W)
    s5_out = s5_mixer(u, a_re, a_im, b_re, b_im, c_re, c_im, d_skip, log_dt, w_glu, b_glu)
    merged = jnp.concatenate([ret_out, s5_out], axis=-1).astype(xn.dtype)
    return merged @ w_out


def dilated_branch(q, k, v, window, dilation):
    B, L, H, dh = q.shape
    band = window // dilation
    ls = L // dilation
    nb = -(-ls // band)
    pad = nb * band - ls

    def to_sub(t):
        t = t.reshape(B, ls, dilation, H, dh).transpose(0, 2, 3, 1, 4)
        t = jnp.pad(t, ((0, 0), (0, 0), (0, 0), (0, pad), (0, 0)))
        return t.reshape(B, dilation, H, nb, band, dh)

    def with_prev(t):
        prev = jnp.pad(t, ((0, 0), (0, 0), (0, 0), (1, 0), (0, 0), (0, 0)))[:, :, :, :-1]
        return jnp.concatenate([prev, t], axis=4)

    qs = to_sub(q)
    kb, vb = with_prev(to_sub(k)), with_prev(to_sub(v))
    s = jnp.einsum('brhnqd,brhnkd->brhnqk', qs, kb) * (dh ** -0.5)
    qi = jnp.arange(band)[:, None]
    kj = jnp.arange(2 * band)[None, :]
    dist = band + qi - kj
    in_band = (dist >= 0) & (dist <= band)
    has_prev = (jnp.arange(nb)[:, None, None] > 0) | (kj >= band)[None]
    valid = in_band[None] & has_prev
    s = jnp.where(valid, s, -jnp.inf)
    m = jnp.max(s, axis=-1, keepdims=True)
    p = jnp.exp(s - m)
    l = jnp.sum(p, axis=-1, keepdims=True)
    o = jnp.einsum('brhnqk,brhnkd->brhnqd', p, vb) / l
    lse = (m + jnp.log(l))[..., 0]
    o = o.reshape(B, dilation, H, nb * band, dh)[:, :, :, :ls].transpose(0, 3, 1, 2, 4).reshape(B, L, H, dh)
    lse = lse.reshape(B, dilation, H, nb * band)[..., :ls].transpose(0, 3, 1, 2).reshape(B, L, H)
    return o, lse


def dilated_attention(xn, w_qkv, w_out):
    B, L, _ = xn.shape
    qkv = (xn @ w_qkv).astype(jnp.float32).reshape(B, L, 3, ATT_HEADS, ATT_DH)
    q, k, v = qkv[:, :, 0], qkv[:, :, 1], qkv[:, :, 2]
    outs, lses = [], []
    for window, dilation in DILATED_BRANCHES:
        o, lse = dilated_branch(q, k, v, window, dilation)
        outs.append(o)
        lses.append(lse)
    wts = jax.nn.softmax(jnp.stack(lses, axis=0), axis=0)
    o = jnp.sum(wts[..., None] * jnp.stack(outs, axis=0), axis=0)
    return o.reshape(B, L, ATT_HEADS * ATT_DH).astype(xn.dtype) @ w_out


def hier_moe(xn, w_group, b_group, w_router, b_router, w_gate, w_up, w_down):
    B, L, D = xn.shape
    t = xn.reshape(B * L, D)
    tf = t.astype(jnp.float32)
    glog = tf @ w_group.astype(jnp.float32) + b_group.astype(jnp.float32)
    gval, gsel = lax.top_k(jax.nn.softmax(glog, axis=-1), 1)
    elog = jnp.einsum('td,dge->tge', tf, w_router.astype(jnp.float32)) + b_router.astype(jnp.float32)
    elog_sel = jnp.take_along_axis(elog, gsel[:, :, None], axis=1)[:, 0]
    top_v, top_i = lax.top_k(elog_sel, TOP_K)
    top_w = jax.nn.softmax(top_v, axis=-1) * gval
    w_in_group = jnp.sum(jax.nn.one_hot(top_i, EXPERTS_PER_GROUP) * top_w[..., None], axis=1)
    combine = jax.nn.one_hot(gsel[:, 0], N_GROUPS)[:, :, None] * w_in_group[:, None, :]
    out = jnp.zeros((B * L, D), jnp.float32)
    for g in range(N_GROUPS):
        h = jax.nn.silu(jnp.einsum('td,edf->tef', t, w_gate[g])) * jnp.einsum('td,edf->tef', t, w_up[g])
        out = out + jnp.einsum('tef,efd->td', h * combine[:, g, :, None], w_down[g])
    return out.reshape(B, L, D).astype(xn.dtype)


def _normal(k, shape, scale):
    return jax.random.normal(k, shape, jnp.float32) * scale


def setup_inputs(seed: int = 0) -> dict:
    key = jax.random.key(seed)
    ks = jax.random.split(key, 32)
    G, P, Hs = S5_GROUPS, S5_STATE, S5_GROUP
    a_im0 = math.pi * jnp.arange(P, dtype=jnp.float32)
    return {
        "x": _normal(ks[0], (BATCH, SEQ, D_MODEL), 1.0),
        "mix_norm": 1.0 + _normal(ks[1], (DEPTH, D_MODEL), 0.01),
        "ffn_norm": 1.0 + _normal(ks[2], (DEPTH, D_MODEL), 0.01),
        "final_norm": 1.0 + _normal(ks[3], (D_MODEL,), 0.01),
        "ab_w_in": _normal(ks[4], (N_EVEN, D_MODEL, AB_IN_W), D_MODEL ** -0.5),
        "ab_w_out": _normal(ks[5], (N_EVEN, AB_OUT_W, D_MODEL), AB_OUT_W ** -0.5),
        "s5_a_re": -0.5 + _normal(ks[6], (N_EVEN, G, P), 0.01),
        "s5_a_im": a_im0[None, None, :] + _normal(ks[7], (N_EVEN, G, P), 0.01),
        "s5_b_re": _normal(ks[8], (N_EVEN, G, P, Hs), Hs ** -0.5),
        "s5_b_im": _normal(ks[9], (N_EVEN, G, P, Hs), Hs ** -0.5),
        "s5_c_re": _normal(ks[10], (N_EVEN, G, Hs, P), P ** -0.5),
        "s5_c_im": _normal(ks[11], (N_EVEN, G, Hs, P), P ** -0.5),
        "s5_d": _normal(ks[12], (N_EVEN, G, Hs), 1.0),
        "s5_log_dt": jax.random.uniform(ks[13], (N_EVEN, G), jnp.float32,
                                        math.log(S5_DT_MIN), math.log(S5_DT_MAX)),
        "s5_w_glu": _normal(ks[14], (N_EVEN, S5_WIDTH, S5_WIDTH), S5_WIDTH ** -0.5),
        "s5_b_glu": _normal(ks[15], (N_EVEN, S5_WIDTH), 0.01),
        "c_w_qkv": _normal(ks[16], (N_ODD, D_MODEL, 3 * ATT_HEADS * ATT_DH), D_MODEL ** -0.5),
        "c_w_out": _normal(ks[17], (N_ODD, ATT_HEADS * ATT_DH, D_MODEL), (ATT_HEADS * ATT_DH) ** -0.5),
        "moe_w_group": _normal(ks[18], (DEPTH, D_MODEL, N_GROUPS), D_MODEL ** -0.5),
        "moe_b_group": _normal(ks[19], (DEPTH, N_GROUPS), 0.01),
        "moe_w_router": _normal(ks[20], (DEPTH, D_MODEL, N_GROUPS, EXPERTS_PER_GROUP), D_MODEL ** -0.5),
        "moe_b_router": _normal(ks[21], (DEPTH, N_GROUPS, EXPERTS_PER_GROUP), 0.01),
        "moe_w_gate": _normal(ks[22], (DEPTH, N_GROUPS, EXPERTS_PER_GROUP, D_MODEL, D_EXPERT), D_MODEL ** -0.5),
        "moe_w_up": _normal(ks[23], (DEPTH, N_GROUPS, EXPERTS_PER_GROUP, D_MODEL, D_EXPERT), D_MODEL ** -0.5),
        "moe_w_down": _normal(ks[24], (DEPTH, N_GROUPS, EXPERTS_PER_GROUP, D_EXPERT, D_MODEL), D_EXPERT ** -0.5),
    }


def reference(x, mix_norm, ffn_norm, final_norm, ab_w_in, ab_w_out, s5_a_re, s5_a_im, s5_b_re, s5_b_im,
              s5_c_re, s5_c_im, s5_d, s5_log_dt, s5_w_glu, s5_b_glu, c_w_qkv, c_w_out,
              moe_w_group, moe_b_group, moe_w_router, moe_b_router, moe_w_gate, moe_w_up, moe_w_down):
    h = x
    pos = jnp.arange(x.shape[1])
    for layer in range(DEPTH):
        i = layer // 2
        hn = rmsnorm(h, mix_norm[layer])
        if layer % 2 == 0:
            mixed = parallel_retention_s5(hn, ab_w_in[i], ab_w_out[i], s5_a_re[i], s5_a_im[i], s5_b_re[i],
                                          s5_b_im[i], s5_c_re[i], s5_c_im[i], s5_d[i], s5_log_dt[i],
                                          s5_w_glu[i], s5_b_glu[i], pos)
        else:
            mixed = dilated_attention(hn, c_w_qkv[i], c_w_out[i])
        h = h + mixed
        h = h + hier_moe(rmsnorm(h, ffn_norm[layer]), moe_w_group[layer], moe_b_group[layer],
                         moe_w_router[layer], moe_b_router[layer], moe_w_gate[layer],
                         moe_w_up[layer], moe_w_down[layer])
    return rmsnorm(h, final_norm)
```

```python
import math
import os
import numpy as np
from contextlib import ExitStack
import concourse.bass as bass
import concourse.mybir as mybir
from concourse.bass_utils import run_bass_kernel_spmd

F32 = mybir.dt.float32
BF16 = mybir.dt.bfloat16
I32 = mybir.dt.int32
AF = mybir.ActivationFunctionType
ALU = mybir.AluOpType
AX = mybir.AxisListType

D = 1024
KC = 8
NCORES = 8
MAGIC = 12582912.0
TWO_PI = 2.0 * math.pi


def ssl(start, n, step=1):
    return slice(start, start + (n - 1) * step + 1, step)


def _isap(x):
    return isinstance(x, bass.AP)


def _region(ap):
    name = ap.tensor.name
    pat = ap.ap
    sz = mybir.dt.size(ap.dtype)
    off = ap.offset
    if str(ap.space) == "DRAM":
        lo = off
        hi = off
        for (s, c) in pat:
            d = (c - 1) * s
            if d < 0:
                lo += d
            else:
                hi += d
        return (name, 0, 1, lo * sz, (hi + 1) * sz)
    pstep, pcnt = pat[0]
    if pstep == 0:
        p0 = 0
        f = off
        pcnt = 128
    else:
        p0 = off // pstep
        f = off - p0 * pstep
    lo = f
    hi = f
    for (s, c) in pat[1:]:
        d = (c - 1) * s
        if d < 0:
            lo += d
        else:
            hi += d
    lo_b = lo * sz
    hi_b = (hi + 1) * sz
    if str(ap.space) == "PSUM":
        return (name, 0, 128, (lo_b // 2048) * 2048, ((hi_b + 2047) // 2048) * 2048)
    return (name, p0, p0 + pcnt, lo_b, hi_b)


class Op:
    __slots__ = ("id", "eng", "fn", "deps", "is_dma", "signal", "idx", "sem", "semval")


class Sched:
    ENGS = ("pe", "act", "dve", "pool", "sp")

    def __init__(self, nc, n_dma_sems=8):
        self.nc = nc
        self.ops = []
        self.by_eng = {e: [] for e in self.ENGS}
        self.recs = {}
        self.n_dma_sems = n_dma_sems
        self.dma_rr = {e: 0 for e in self.ENGS}
        self.dma_rr["cc"] = 0
        self.dma_last = {}

    def _touch(self, op, aps, is_write):
        deps = op.deps
        for ap in aps:
            if ap is None or not _isap(ap):
                continue
            name, p0, p1, f0, f1 = _region(ap)
            lst = self.recs.get(name, [])
            keep = []
            for r in lst:
                rp0, rp1, rf0, rf1, rid, rw, reng = r
                ov = not (rp1 <= p0 or p1 <= rp0 or rf1 <= f0 or f1 <= rf0)
                if ov and (is_write or rw) and rid != op.id:
                    deps.add(rid)
                if is_write and rp0 >= p0 and rp1 <= p1 and rf0 >= f0 and rf1 <= f1 and rid != op.id:
                    continue
                if (not is_write) and (not rw) and reng == op.eng and (not op.is_dma) \
                        and (not self.ops[rid].is_dma) and (rp0, rp1, rf0, rf1) == (p0, p1, f0, f1):
                    continue
                keep.append(r)
            keep.append((p0, p1, f0, f1, op.id, is_write, op.eng))
            self.recs[name] = keep

    def add(self, eng, fn, reads=(), writes=(), dma=False):
        op = Op()
        op.id = len(self.ops)
        op.eng = eng
        op.fn = fn
        op.deps = set()
        op.is_dma = bool(dma)
        op.signal = bool(dma)
        op.sem = None
        op.semval = None
        self.ops.append(op)
        self._touch(op, reads, False)
        self._touch(op, writes, True)
        if dma == "cc":
            op.sem = ("cc", self.dma_rr["cc"])
            self.dma_rr["cc"] += 1
            op.semval = 1
            self.dma_last[op.sem] = op
        elif dma:
            slot = (eng, self.dma_rr[eng] % self.n_dma_sems)
            self.dma_rr[eng] += 1
            prev = self.dma_last.get(slot)
            if prev is not None:
                op.deps.add(prev.id)
                op.semval = prev.semval + 16
            else:
                op.semval = 16
            op.sem = slot
            self.dma_last[slot] = op
        op.idx = len(self.by_eng[eng])
        self.by_eng[eng].append(op)
        return op

    def plan(self):
        ops = self.ops
        for op in ops:
            for d in op.deps:
                dop = ops[d]
                if dop.is_dma:
                    continue
                if dop.eng == "pe" and op.eng == "pe" and not op.is_dma:
                    continue
                dop.signal = True
        for e in self.ENGS:
            c = 0
            for op in self.by_eng[e]:
                if (not op.is_dma) and op.signal:
                    c += 1
                    op.semval = c
        know = {e: {f: 0 for f in self.ENGS} for e in self.ENGS}
        know_dma = {e: set() for e in self.ENGS}
        snap = {}
        waits = {}
        for op in ops:
            e = op.eng
            w = []
            need = {}
            for d in sorted(op.deps):
                dop = ops[d]
                if dop.is_dma:
                    if d not in know_dma[e]:
                        w.append(("dma", dop.sem, dop.semval))
                        know_dma[e].add(d)
                    continue
                if dop.eng == "pe" and e == "pe" and not op.is_dma:
                    continue
                if know[e][dop.eng] >= dop.semval:
                    continue
                need[dop.eng] = max(need.get(dop.eng, 0), dop.semval)
            for f, v in sorted(need.items(), key=lambda kv: -kv[1]):
                if know[e][f] >= v:
                    continue
                w.append(("eng", f, v))
                know[e][f] = v
                sn = snap.get((f, v))
                if sn is not None:
                    for g, gv in sn[0].items():
                        if know[e][g] < gv:
                            know[e][g] = gv
                    know_dma[e] |= sn[1]
            waits[op.id] = w
            if (not op.is_dma) and op.signal:
                snap[(e, op.semval)] = (dict(know[e]), set(know_dma[e]))
        return waits

    def run_block(self, final_wait_ops=()):
        nc = self.nc
        waits = self.plan()
        with ExitStack() as st:
            esem = {e: st.enter_context(nc.semaphore("s_" + e)) for e in self.ENGS}
            dsem = {}
            for (e, k) in sorted(self.dma_last.keys()):
                dsem[(e, k)] = st.enter_context(nc.semaphore("d_%s%d" % (e, k)))
            block = st.enter_context(nc.Block())

            def body(ename, h):
                for op in self.by_eng[ename]:
                    for (kind, a, v) in waits[op.id]:
                        h.wait_ge(dsem[a] if kind == "dma" else esem[a], v)
                    ins = op.fn(h)
                    if op.is_dma and op.sem[0] == "cc":
                        ins.then_inc(dsem[op.sem])
                    elif op.is_dma:
                        ins.then_inc(dsem[op.sem], 16)
                    elif op.signal:
                        ins.then_inc(esem[ename], 1)
                for fop in final_wait_ops:
                    if fop.eng == ename:
                        h.wait_ge(dsem[fop.sem], fop.semval)

            @block.tensor
            def _(h):
                body("pe", h)

            @block.scalar
            def _(h):
                body("act", h)

            @block.vector
            def _(h):
                body("dve", h)

            @block.gpsimd
            def _(h):
                body("pool", h)

            @block.sync
            def _(h):
                body("sp", h)


class Arena:
    def __init__(self, t, nwords, base=0):
        self.t = t
        self.n = base + nwords
        self.top = base

    def mark(self):
        return self.top

    def release(self, m):
        self.top = m

    def alloc(self, shape, dt):
        sz = mybir.dt.size(dt)
        n = 1
        for s in shape[1:]:
            n *= s
        words = (n * sz + 3) // 4
        words = (words + 7) // 8 * 8
        assert self.top + words <= self.n, ("SBUF arena overflow", self.top, words, self.n)
        ap = self.t[0:shape[0], self.top:self.top + words]
        self.top += words
        if dt != F32:
            ap = ap.bitcast(dt)
        ap = ap[:, 0:n]
        if len(shape) > 2:
            names = " ".join("d%d" % i for i in range(len(shape) - 1))
            kw = {"d%d" % i: shape[i + 1] for i in range(len(shape) - 1)}
            ap = ap.rearrange("p (%s) -> p %s" % (names, names), **kw)
        return ap


class KB:
    def __init__(self, nc, arena, psum):
        self.nc = nc
        self.S = Sched(nc)
        self.A = arena
        self.psum = psum

    def bank(self, b, n=512, dt=F32, off=0):
        ap = self.psum[:, b * 512:(b + 1) * 512]
        if dt != F32:
            ap = ap.bitcast(dt)
        return ap[:, off:off + n]

    def mm(self, out, lhsT, rhs, start=True, stop=True):
        rd = [lhsT, rhs] + ([] if start else [out])
        return self.S.add("pe", lambda e: e.matmul(out, lhsT=lhsT, rhs=rhs, start=start, stop=stop), rd, [out])

    def tr(self, out, in_, ident):
        return self.S.add("pe", lambda e: e.transpose(out=out, in_=in_, identity=ident), [in_, ident], [out])

    def act(self, out, in_, func, scale=None, bias=None, accum=None):
        kw = {}
        rd = [in_]
        wr = [out]
        if scale is not None:
            kw["scale"] = scale
            rd.append(scale)
        if bias is not None:
            kw["bias"] = bias
            rd.append(bias)
        if accum is not None:
            kw["accum_out"] = accum
            wr.append(accum)
        return self.S.add("act", lambda e: e.activation(out=out, in_=in_, func=func, **kw), rd, wr)

    def tt(self, eng, out, a, b, op):
        return self.S.add(eng, lambda e: e.tensor_tensor(out=out, in0=a, in1=b, op=op), [a, b], [out])

    def ts(self, eng, out, a, s1, op0, s2=None, op1=None):
        if op1 is None:
            return self.S.add(eng, lambda e: e.tensor_scalar(out=out, in0=a, scalar1=s1, scalar2=None, op0=op0), [a, s1], [out])
        return self.S.add(eng, lambda e: e.tensor_scalar(out=out, in0=a, scalar1=s1, scalar2=s2, op0=op0, op1=op1), [a, s1, s2], [out])

    def stt(self, out, a, scalar, b, op0, op1):
        return self.S.add("dve", lambda e: e.scalar_tensor_tensor(out=out, in0=a, scalar=scalar, in1=b, op0=op0, op1=op1), [a, scalar, b], [out])

    def cp(self, eng, out, in_):
        if eng == "act":
            return self.S.add("act", lambda e: e.copy(out=out, in_=in_), [in_], [out])
        return self.S.add(eng, lambda e: e.tensor_copy(out=out, in_=in_), [in_], [out])

    def memset(self, eng, ap, val):
        return self.S.add(eng, lambda e: e.memset(ap, val), [], [ap])

    def dma(self, q, out, in_, **kw):
        return self.S.add(q, lambda e: e.dma_start(out=out, in_=in_, **kw), [in_], [out], dma=True)

    def recip(self, out, in_):
        return self.S.add("dve", lambda e: e.reciprocal(out=out, in_=in_), [in_], [out])

    def iota(self, out, pattern, base, cm):
        return self.S.add("pool", lambda e: e.iota(out, pattern=pattern, base=base, channel_multiplier=cm), [], [out])

    def asel(self, out, in_, pattern, op, fill, base, cm):
        return self.S.add("pool", lambda e: e.affine_select(out=out, in_=in_, pattern=pattern, compare_op=op, fill=fill, base=base, channel_multiplier=cm), [in_], [out])


RG = [[0, 1], [2, 3], [4, 5], [6, 7]]
LN_G = [math.log1p(-2.0 ** (-5 - hh)) for hh in range(4)]
LN_DK = math.log(128.0 ** -0.5)


class Prog:
    def __init__(self, NCH=16, stages=("l0mix", "moe0", "l1mix", "moe1", "final"), dumps=(), n_cores=8):
        self.NCH = NCH
        self.T = NCH * 128
        self.NTB = self.T // 512
        self.stages = tuple(stages)
        self.dumps = set(dumps)
        self.dump_t = {}
        self.n_cores = n_cores
        self.rg = RG[: n_cores // 2]
        self.nc = bass.Bass("TRN2", target_bir_lowering=False)
        self.out_ops = []
        self.build()

    def din(self, name, shape):
        return self.nc.dram_tensor(name, list(shape), F32, kind="ExternalInput").ap()

    def dint(self, name, shape, dt):
        return self.nc.dram_tensor(name, list(shape), dt).ap()

    def dump(self, name, ap, dram_shape, pattern=None, **kw):
        if name not in self.dumps:
            return
        t = self.nc.dram_tensor("dbg_" + name, list(dram_shape), ap.dtype, kind="ExternalOutput").ap()
        self.dump_t[name] = (list(dram_shape), ap.dtype)
        dst = t if pattern is None else t.rearrange(pattern, **kw)
        self.out_ops.append(self.K.dma("sp", dst, ap))

    def build(self):
        nc = self.nc
        T = self.T
        i = self.inp = {}
        for name, shape in [("x", [T, D]), ("flag", [128, 1]), ("mix_norm", [2, D]), ("ffn_norm", [2, D]),
                            ("final_norm", [D]), ("ab_w_in", [1, D, 2560]), ("ab_w_out", [1, D, D]),
                            ("s5_a_re", [1, 32, 64]), ("s5_a_im", [1, 32, 64]), ("s5_b_re", [1, 32, 64, 16]),
                            ("s5_b_im", [1, 32, 64, 16]), ("s5_c_re", [1, 32, 16, 64]), ("s5_c_im", [1, 32, 16, 64]),
                            ("s5_d", [1, 32, 16]), ("s5_log_dt", [1, 32]), ("s5_w_glu", [1, 512, 512]),
                            ("s5_b_glu", [1, 512]), ("c_w_qkv", [1, D, 3072]), ("c_w_out", [1, D, D]),
                            ("moe_w_group", [2, D, 4]), ("moe_b_group", [2, 4]), ("moe_w_router", [2, D, 4, 4]),
                            ("moe_b_router", [2, 4, 4]), ("moe_w_gate", [2, 4, 4, D, 512]),
                            ("moe_w_up", [2, 4, 4, D, 512]), ("moe_w_down", [2, 4, 4, 512, D])]:
            i[name] = self.din(name, shape)
        self.y_out = nc.dram_tensor("y", [T, D], F32, kind="ExternalOutput").ap()
        ARENA_WORDS = 51200
        with ExitStack() as st:
            arena_t = st.enter_context(nc.sbuf_tensor("arena", [128, ARENA_WORDS], F32))
            psum_t = st.enter_context(nc.psum_tensor("psum", [128, 4096], F32))
            self.A = A = Arena(arena_t, ARENA_WORDS)
            self.K = K = KB(nc, A, psum_t)
            self.setup_consts()
            if "l0mix" in self.stages:
                self.l0mix()
            else:
                K.dma("sp", self.h, i["x"].rearrange("(c p) d -> p c d", p=128))
            self.dump("h0m", self.h, [T, D], "(c p) d -> p c d", p=128)
            if "moe0" in self.stages:
                self.moe(0)
            self.dump("h0", self.h, [T, D], "(c p) d -> p c d", p=128)
            if "l1mix" in self.stages:
                self.l1mix()
            self.dump("h1m", self.h, [T, D], "(c p) d -> p c d", p=128)
            if "moe1" in self.stages:
                self.moe(1)
            self.dump("h1", self.h, [T, D], "(c p) d -> p c d", p=128)
            self.final()
            K.S.run_block(final_wait_ops=self.out_ops)

    def setup_consts(self):
        K, A = self.K, self.A
        self.ident_f = A.alloc([128, 128], F32)
        self.ident_b = A.alloc([128, 128], BF16)
        self.ones_b = A.alloc([128, 128], BF16)
        self.flag = A.alloc([128, 1], F32)
        self.gB = A.alloc([128, D], F32)
        self.stats = A.alloc([128, 32], F32)
        self.pidx = A.alloc([128, 1], F32)
        self.signc = A.alloc([128, 1], F32)
        self.cst = A.alloc([128, 16], F32)
        K.memset("pool", self.ident_f, 1.0)
        K.asel(self.ident_f, self.ident_f, [[-1, 128]], ALU.is_equal, 0.0, 0, 1)
        K.cp("dve", self.ident_b, self.ident_f)
        K.memset("pool", self.ones_b, 1.0)
        self.pswap = A.alloc([128, 128], F32)
        K.cp("dve", self.pswap[:, 0:64], self.ident_f[:, 64:128])
        K.cp("dve", self.pswap[:, 64:128], self.ident_f[:, 0:64])
        K.dma("sp", self.flag, self.inp["flag"])
        pi = A.alloc([128, 1], I32)
        K.iota(pi, [[0, 1]], 0, 1)
        K.cp("dve", self.pidx, pi)
        K.memset("pool", self.signc[0:64, :], -1.0)
        K.memset("pool", self.signc[64:128, :], 1.0)
        for hh in range(4):
            K.memset("pool", self.cst[:, hh:hh + 1], LN_G[hh])
            K.memset("pool", self.cst[:, 4 + hh:5 + hh], 127.0 * LN_G[hh] + LN_DK)
        K.memset("pool", self.cst[:, 8:9], LN_DK)
        K.memset("pool", self.cst[:, 9:10], 0.0)
        K.memset("pool", self.cst[:, 10:11], math.pi / 2)
        self.stat_rr = 0
        h_base = A.mark()
        self.h = A.alloc([128, 16, D], F32)[:, 0:self.NCH, :]
        self.HA = Arena(A.t, 16 * D, base=h_base)
        self.xnT = A.alloc([128, KC, self.T], BF16)
        self.NST = 3
        self.stage = [A.alloc([128, 2048], F32) for _ in range(self.NST)]
        self.st_rr = 0
        self.base_mark = A.mark()

    def stat_slot(self):
        s = self.stat_rr % 8
        self.stat_rr += 1
        return self.stats[:, s * 4:(s + 1) * 4]

    def load_gamma(self, vec_ap):
        self.K.dma("sp", self.gB, vec_ap.partition_broadcast(128))

    def rms(self, src, out, junk):
        K = self.K
        ss = self.stat_slot()
        K.act(junk, src, AF.Square, accum=ss[:, 0:1])
        K.ts("dve", ss[:, 1:2], ss[:, 0:1], 1.0 / D, ALU.mult, 1e-6, ALU.add)
        K.act(ss[:, 2:3], ss[:, 1:2], AF.Sqrt)
        K.recip(ss[:, 3:4], ss[:, 2:3])
        K.stt(out, src, ss[:, 3:4], self.gB, ALU.mult, ALU.mult)

    def wload(self, dst, src, shape, cast_eng="pool"):
        a, b = shape
        assert a * b <= 2048
        sl = self.stage[self.st_rr % self.NST]
        self.st_rr += 1
        v = sl[:, 0:a * b].rearrange("p (a b) -> p a b", a=a)
        self.K.dma("sp", v, src)
        self.K.cp(cast_eng, dst, v)

    def to_xnT(self, xn_bf, c, bk):
        K = self.K
        pT = K.bank(bk, 1024, BF16)
        for k in range(KC):
            K.tr(pT[:, k * 128:(k + 1) * 128], xn_bf[:, k * 128:(k + 1) * 128], self.ident_b)
        K.cp("act", self.xnT[:, :, c * 128:(c + 1) * 128], pT.rearrange("p (k t) -> p k t", k=KC))

    def sin_rr(self, out, ang, tmp):
        K = self.K
        K.ts("dve", tmp, ang, 1.0 / TWO_PI, ALU.mult, MAGIC, ALU.add)
        K.ts("dve", tmp, tmp, MAGIC, ALU.subtract, -TWO_PI, ALU.mult)
        K.tt("dve", tmp, ang, tmp, ALU.add)
        K.ts("dve", tmp, tmp, -3.14159, ALU.max, 3.14159, ALU.min)
        K.act(out, tmp, AF.Sin)

    def final(self):
        K, A = self.K, self.A
        m = A.mark()
        if "final" in self.stages:
            self.load_gamma(self.inp["final_norm"])
        yo = [A.alloc([128, D], F32) for _ in range(2)]
        junk = A.alloc([128, D], BF16)
        for c in range(self.NCH):
            if "final" in self.stages:
                self.rms(self.h[:, c, :], yo[c % 2], junk)
                src = yo[c % 2]
            else:
                src = self.h[:, c, :]
            self.out_ops.append(K.dma("sp", self.y_out[c * 128:(c + 1) * 128, :], src))
        A.release(m)

    def l0mix(self):
        K, A, HA, NCH, T, NTB = self.K, self.A, self.HA, self.NCH, self.T, self.NTB
        nc = self.nc
        inp = self.inp
        xnT = self.xnT
        m0 = A.mark()
        hm0 = HA.mark()
        uT = A.alloc([128, 4, T], BF16)
        mA = A.mark()
        qT = A.alloc([128, 4, T], BF16)
        kT = A.alloc([128, 4, T], BF16)
        vtm = A.alloc([128, NCH, 512], BF16)
        sg_d = self.dint("sg_d", [NCH, 128, 512], BF16)
        sgb = [A.alloc([128, 512], BF16) for _ in range(2)]
        m1 = A.mark()
        hm1 = HA.mark()
        if "s5" in self.stages:
            self.s5_setup()

        xc = [HA.alloc([128, D], F32) for _ in range(2)]
        xnb = [HA.alloc([128, D], BF16) for _ in range(2)]
        junk = HA.alloc([128, D], BF16)
        self.load_gamma(inp["mix_norm"][0])
        for c in range(NCH):
            K.dma("sp", xc[c % 2], inp["x"][c * 128:(c + 1) * 128, :])
            self.rms(xc[c % 2], xnb[c % 2], junk)
            self.to_xnT(xnb[c % 2], c, c % 2)
        HA.release(hm1)

        cosT = HA.alloc([128, T], F32)
        sinST = HA.alloc([128, T], F32)
        hm2 = HA.mark()
        ang = HA.alloc([128, T], F32)
        tmp = HA.alloc([128, T], F32)
        pos_i = HA.alloc([128, T], I32)
        ji = HA.alloc([128, 1], I32)
        jf = HA.alloc([128, 1], F32)
        inv = HA.alloc([128, 1], F32)
        offc = HA.alloc([128, 1], F32)
        K.iota(pos_i, [[1, T]], 0, 0)
        K.cp("dve", tmp, pos_i)
        K.iota(ji[0:64, :], [[0, 1]], 0, 1)
        K.iota(ji[64:128, :], [[0, 1]], 0, 1)
        K.cp("dve", jf, ji)
        K.act(inv, jf, AF.Exp, scale=-math.log(10000.0) / 64.0)
        K.ts("dve", offc, self.flag, float(T), ALU.mult)
        K.ts("dve", ang, tmp, offc, ALU.add, inv, ALU.mult)
        self.sin_rr(sinST, ang, tmp)
        K.ts("dve", sinST, sinST, self.signc, ALU.mult)
        K.ts("dve", ang, ang, math.pi / 2, ALU.add)
        self.sin_rr(cosT, ang, tmp)
        HA.release(hm2)

        Win = inp["ab_w_in"][0].rearrange("(k p) n -> p k n", p=128)
        wn = HA.alloc([128, KC, 512], BF16)
        ws = HA.alloc([128, KC, 512], BF16)
        t1 = [HA.alloc([128, 512], F32) for _ in range(2)]
        t2 = [HA.alloc([128, 512], F32) for _ in range(2)]
        wvg = HA.alloc([128, KC, 1024], BF16)
        it = 0
        for base, dst in ((0, qT), (512, kT)):
            for j in range(2):
                self.wload(wn[:, 4 * j:4 * j + 4, :], Win[:, 4 * j:4 * j + 4, base:base + 512], (4, 512))
            wn5 = wn.rearrange("p k (h two q) -> p k h two q", h=4, two=2)
            ws5 = ws.rearrange("p k (h two q) -> p k h two q", h=4, two=2)
            K.cp("pool", ws5[:, :, :, 0, :], wn5[:, :, :, 1, :])
            K.cp("act", ws5[:, :, :, 1, :], wn5[:, :, :, 0, :])
            for hd in range(4):
                hsl = slice(hd * 128, (hd + 1) * 128)
                for tb in range(NTB):
                    pa = K.bank(2 + it % 2)
                    pb = K.bank(4 + it % 2)
                    tsl = slice(tb * 512, (tb + 1) * 512)
                    for kc in range(KC):
                        K.mm(pa, wn[:, kc, hsl], xnT[:, kc, tsl], kc == 0, kc == KC - 1)
                    for kc in range(KC):
                        K.mm(pb, ws[:, kc, hsl], xnT[:, kc, tsl], kc == 0, kc == KC - 1)
                    K.tt("dve", t1[it % 2], pa, cosT[:, tsl], ALU.mult)
                    K.tt("dve", t2[it % 2], pb, sinST[:, tsl], ALU.mult)
                    K.tt("pool" if it % 2 == 0 else "dve", dst[:, hd, tsl], t1[it % 2], t2[it % 2], ALU.add)
                    it += 1
        for j in range(2):
            self.wload(wn[:, 4 * j:4 * j + 4, :], Win[:, 4 * j:4 * j + 4, 2048:2560], (4, 512))
        for cb in range(4):
            for tb in range(NTB):
                pa = K.bank(2 + it % 2)
                tsl = slice(tb * 512, (tb + 1) * 512)
                for kc in range(KC):
                    K.mm(pa, wn[:, kc, cb * 128:(cb + 1) * 128], xnT[:, kc, tsl], kc == 0, kc == KC - 1)
                K.cp("act", uT[:, cb, tsl], pa)
                it += 1
        for j in range(4):
            self.wload(wvg[:, :, j * 256:(j + 1) * 256], Win[:, :, 1024 + j * 256:1024 + (j + 1) * 256], (KC, 256))
        for c in range(NCH):
            pv = K.bank(2 + c % 2)
            pg = K.bank(4 + c % 2)
            csl = slice(c * 128, (c + 1) * 128)
            for kc in range(KC):
                K.mm(pv, xnT[:, kc, csl], wvg[:, kc, 0:512], kc == 0, kc == KC - 1)
            for kc in range(KC):
                K.mm(pg, xnT[:, kc, csl], wvg[:, kc, 512:1024], kc == 0, kc == KC - 1)
            K.cp("act", vtm[:, c, :], pv)
            K.act(sgb[c % 2], pg, AF.Silu)
            K.dma("sp", sg_d[c], sgb[c % 2])
        HA.release(hm1)
        self.dump("qT", qT, [4, 128, T], "h p t -> p h t")
        self.dump("kT", kT, [4, 128, T], "h p t -> p h t")
        self.dump("uT", uT, [4, 128, T], "h p t -> p h t")

        decT = HA.alloc([128, 4, 128], F32)
        xi = HA.alloc([128, 4], F32)
        zeta = HA.alloc([128, 4], F32)
        Sst = HA.alloc([128, 4, 128], F32)
        Sin = HA.alloc([128, 4, 128], F32)
        Sb = HA.alloc([128, 4, 128], BF16)
        kz = [HA.alloc([128, 4, 128], BF16) for _ in range(2)]
        dfi = HA.alloc([128, 128], I32)
        dff = HA.alloc([128, 128], F32)
        K.iota(dfi, [[1, 128]], 0, -1)
        K.cp("dve", dff, dfi)
        for hd in range(4):
            K.act(decT[:, hd, :], dff, AF.Exp, scale=LN_G[hd], bias=self.cst[:, 8:9])
            K.asel(decT[:, hd, :], decT[:, hd, :], [[1, 128]], ALU.is_ge, 0.0, 0, -1)
            K.act(xi[:, hd:hd + 1], self.pidx, AF.Exp, scale=LN_G[hd], bias=self.cst[:, hd:hd + 1])
            K.act(zeta[:, hd:hd + 1], self.pidx, AF.Exp, scale=-LN_G[hd], bias=self.cst[:, 4 + hd:5 + hd])
        K.memset("pool", Sst, 0.0)
        g128 = [math.exp(128.0 * LN_G[hd]) for hd in range(4)]

        def state_update(n):
            kzb = kz[n % 2]
            pz = K.bank(0, 512, BF16)
            csl = slice(n * 128, (n + 1) * 128)
            for hd in range(4):
                K.tr(pz[:, hd * 128:(hd + 1) * 128], kT[:, hd, csl], self.ident_b)
            for hd in range(4):
                K.act(kzb[:, hd, :], pz[:, hd * 128:(hd + 1) * 128], AF.Copy, scale=zeta[:, hd:hd + 1])
            pkv = K.bank(1)
            for hd in range(4):
                K.mm(pkv[:, hd * 128:(hd + 1) * 128], kzb[:, hd, :], vtm[:, n, hd * 128:(hd + 1) * 128])
            for hd in range(4):
                K.stt(Sst[:, hd, :], Sst[:, hd, :], g128[hd], pkv[:, hd * 128:(hd + 1) * 128], ALU.mult, ALU.add)

        do_s5p1 = "s5" in self.stages and os.environ.get("KSTOP") != "setup"
        if do_s5p1:
            s5_step, s5_fin = self.s5_pass1_begin(uT)
        for n in range(NCH):
            state_update(n)
            if do_s5p1:
                s5_step(n)
        NW = 512 + 32
        st_loc = self.dint("st_loc", [128, NW], F32)
        st_all = self.dint("st_all", [256, NW], F32)
        K.dma("sp", st_loc[:, 0:512], Sst.rearrange("p h e -> p (h e)"))
        if do_s5p1:
            s5_fin(st_loc)
        else:
            zz = HA.alloc([128, 32], F32)
            K.memset("pool", zz, 0.0)
            K.dma("sp", st_loc[:, 512:544], zz)
        K.S.add("pool", lambda e: e.collective_compute("AllGather", ALU.bypass, replica_groups=self.rg,
                                                       ins=[st_loc.opt()], outs=[st_all.opt()]),
                [st_loc], [st_all], dma="cc")
        K.dma("sp", Sin.rearrange("p h e -> p (h e)"), st_all[0:128, 0:512])
        K.ts("dve", Sst.rearrange("p h e -> p (h e)"), Sin.rearrange("p h e -> p (h e)"), self.flag, ALU.mult)

        PT = HA.alloc([128, 4, 128], BF16)
        tmpo = HA.alloc([128, 512], F32)
        o = HA.alloc([128, 512], F32)
        on = HA.alloc([128, 512], F32)
        ret = HA.alloc([128, 512], BF16)
        bst = HA.alloc([128, 24], F32)
        mv = HA.alloc([128, 8], F32)
        rs = HA.alloc([128, 4], F32)
        nb = HA.alloc([128, 4], F32)
        mv3 = mv.rearrange("p (h two) -> p h two", two=2)
        mergedT = xnT
        for n in range(NCH):
            csl = slice(n * 128, (n + 1) * 128)
            K.cp("act", Sb, Sst)
            ps_s = K.bank(2)
            for hd in range(4):
                K.mm(ps_s[:, hd * 128:(hd + 1) * 128], kT[:, hd, csl], qT[:, hd, csl])
            K.tt("dve", PT.rearrange("p h c -> p (h c)"), ps_s, decT.rearrange("p h c -> p (h c)"), ALU.mult)
            pO1 = K.bank(3)
            pO2 = K.bank(4)
            for hd in range(4):
                K.mm(pO1[:, hd * 128:(hd + 1) * 128], PT[:, hd, :], vtm[:, n, hd * 128:(hd + 1) * 128])
            for hd in range(4):
                K.mm(pO2[:, hd * 128:(hd + 1) * 128], qT[:, hd, csl], Sb[:, hd, :])
            for hd in range(4):
                K.act(tmpo[:, hd * 128:(hd + 1) * 128], pO2[:, hd * 128:(hd + 1) * 128], AF.Copy, scale=xi[:, hd:hd + 1])
            K.tt("dve", o, tmpo, pO1, ALU.add)
            for hd in range(4):
                oh = o[:, hd * 128:(hd + 1) * 128]
                bs = bst[:, hd * 6:(hd + 1) * 6]
                K.S.add("dve", lambda e, oh=oh, bs=bs: e.bn_stats(out=bs, in_=oh), [oh], [bs])
                mvh = mv[:, hd * 2:(hd + 1) * 2]
                K.S.add("dve", lambda e, mvh=mvh, bs=bs: e.bn_aggr(out=mvh, in_=bs), [bs], [mvh])
            K.ts("dve", rs, mv3[:, :, 1], 1e-6, ALU.add)
            K.act(rs, rs, AF.Sqrt)
            K.recip(rs, rs)
            K.stt(nb, mv3[:, :, 0], -1.0, rs, ALU.mult, ALU.mult)
            for hd in range(4):
                K.ts("dve", on[:, hd * 128:(hd + 1) * 128], o[:, hd * 128:(hd + 1) * 128], rs[:, hd:hd + 1], ALU.mult,
                     nb[:, hd:hd + 1], ALU.add)
            K.dma("sp", sgb[n % 2], sg_d[n])
            K.tt("pool", ret, on, sgb[n % 2], ALU.mult)
            pR = K.bank(5, 512, BF16)
            for hd in range(4):
                K.tr(pR[:, hd * 128:(hd + 1) * 128], ret[:, hd * 128:(hd + 1) * 128], self.ident_b)
            K.cp("act", mergedT[:, 0:4, csl], pR.rearrange("p (h t) -> p h t", h=4))
            if n + 1 < NCH:
                state_update(n)
        self.dump("retT", mergedT[:, 0:4, :], [4, 128, T], "h p t -> p h t")
        HA.release(hm1)
        A.release(mA)
        if "s5" in self.stages and os.environ.get("KSTOP") not in ("setup", "pass1"):
            self.s5_pass2(uT, st_all, mergedT)
        else:
            zt = HA.alloc([128, 4, 128], BF16)
            K.memset("pool", zt, 0.0)
            for n in range(NCH):
                K.cp("pool", mergedT[:, 4:8, n * 128:(n + 1) * 128], zt)
        self.dump("mergedT", mergedT, [8, 128, T], "h p t -> p h t")
        A.release(m0)
        HA.release(hm0)

        m2 = A.mark()
        wo = A.alloc([128, KC, D], BF16)
        Wout = inp["ab_w_out"][0].rearrange("(k p) n -> p k n", p=128)
        for j in range(4):
            self.wload(wo[:, :, j * 256:(j + 1) * 256], Wout[:, :, j * 256:(j + 1) * 256], (KC, 256))
        for c in range(NCH):
            csl = slice(c * 128, (c + 1) * 128)
            K.dma("sp", self.h[:, c, :], inp["x"][c * 128:(c + 1) * 128, :])
            for dh in range(2):
                po = K.bank(6 + dh)
                dsl = slice(dh * 512, (dh + 1) * 512)
                for fb in range(8):
                    K.mm(po, mergedT[:, fb, csl], wo[:, fb, dsl], fb == 0, fb == 7)
                K.tt("dve", self.h[:, c, dsl], po, self.h[:, c, dsl], ALU.add)
        A.release(m2)

    def cmul(self, Y, Ac, Bc, X, tA, tB):
        K = self.K
        psw = K.bank(4)[:, 0:32]
        K.mm(psw, self.pswap, X)
        K.tt("dve", tA, X, Ac, ALU.mult)
        K.tt("dve", tB, psw, Bc, ALU.mult)
        K.tt("dve", Y, tA, tB, ALU.add)

    def s5_setup(self):
        K, HA = self.K, self.HA
        inp = self.inp
        d = self.s5d = {}
        for nm, n, dt in [("TA", 2048, BF16), ("TB", 2048, BF16), ("TBn", 2048, BF16), ("BB", 4096, BF16),
                          ("A2", 4096, BF16), ("B2", 4096, BF16), ("C1", 4096, BF16), ("C2", 4096, BF16),
                          ("L", 192, F32)]:
            d[nm] = self.dint("s5_" + nm, [128, n], dt)
        hm = HA.mark()

        def tl():
            return HA.alloc([128, 32], F32)

        X = HA.alloc([32, 256], F32)
        a_re = inp["s5_a_re"][0]
        a_im = inp["s5_a_im"][0]
        K.dma("sp", X[:, 0:64], a_re)
        K.dma("sp", X[:, 64:128], a_re)
        K.dma("sp", X[:, 128:192], a_im)
        K.dma("sp", X[:, 192:256], a_im)
        pb = K.bank(6)
        K.tr(pb[:, 0:32], X[:, 0:128], self.ident_f[0:32, 0:32])
        K.tr(pb[:, 32:64], X[:, 128:256], self.ident_f[0:32, 0:32])
        are2, aim2, ldt, dt, zr, zi = tl(), tl(), tl(), tl(), tl(), tl()
        K.cp("act", are2, pb[:, 0:32])
        K.cp("act", aim2, pb[:, 32:64])
        K.dma("sp", ldt, inp["s5_log_dt"][0].partition_broadcast(128))
        K.act(dt, ldt, AF.Exp)
        K.tt("dve", zr, are2, dt, ALU.mult)
        K.tt("dve", zi, aim2, dt, ALU.mult)
        self.s5_zr, self.s5_zi = zr, zi
        Lc = HA.alloc([128, 6, 32], F32)
        tmp, angk, mg, sn, cs = tl(), tl(), tl(), tl(), tl()

        def lam_pow(kk, Ac, Bc):
            K.ts("dve", angk, zi, float(kk), ALU.mult)
            self.sin_rr(sn, angk, tmp)
            K.ts("dve", angk, angk, math.pi / 2, ALU.add)
            self.sin_rr(cs, angk, tmp)
            K.act(mg, zr, AF.Exp, scale=float(kk))
            K.tt("dve", Ac, mg, cs, ALU.mult)
            K.stt(Bc, mg, self.signc, sn, ALU.mult, ALU.mult)

        for j, kk in enumerate((1, 127, 128)):
            lam_pow(kk, Lc[:, 2 * j, :], Lc[:, 2 * j + 1, :])
        K.dma("sp", d["L"], Lc.rearrange("p a g -> p (a g)"))
        lre, lim_s = Lc[:, 0, :], Lc[:, 1, :]
        nr, ni, den, cre, cis, t3 = tl(), tl(), tl(), tl(), tl(), tl()
        K.ts("dve", nr, lre, -1.0, ALU.add)
        K.ts("dve", ni, lim_s, self.signc, ALU.mult)
        K.tt("dve", den, are2, are2, ALU.mult)
        K.tt("dve", t3, aim2, aim2, ALU.mult)
        K.tt("dve", den, den, t3, ALU.add)
        K.recip(den, den)
        K.tt("dve", cre, nr, are2, ALU.mult)
        K.tt("dve", t3, ni, aim2, ALU.mult)
        K.tt("dve", cre, cre, t3, ALU.add)
        K.tt("dve", cre, cre, den, ALU.mult)
        K.tt("dve", cis, ni, are2, ALU.mult)
        K.tt("dve", t3, nr, aim2, ALU.mult)
        K.tt("dve", cis, cis, t3, ALU.subtract)
        K.tt("dve", cis, cis, den, ALU.mult)
        K.ts("dve", cis, cis, self.signc, ALU.mult)
        hm_small = HA.mark()
        M1 = HA.alloc([128, 32, 16], F32)
        M2 = HA.alloc([128, 32, 16], F32)
        bre = inp["s5_b_re"][0].rearrange("g p h -> p g h")
        bim = inp["s5_b_im"][0].rearrange("g p h -> p g h")
        K.dma("sp", M1[0:64], bre)
        K.dma("sp", M1[64:128], bim)
        K.dma("sp", M2[0:64], bim)
        K.dma("sp", M2[64:128], bre)
        bbX = HA.alloc([128, 32, 16], F32)
        tmb = HA.alloc([128, 32, 16], F32)
        K.tt("dve", bbX, M1, cre.unsqueeze(2).to_broadcast([128, 32, 16]), ALU.mult)
        K.tt("dve", tmb, M2, cis.unsqueeze(2).to_broadcast([128, 32, 16]), ALU.mult)
        K.tt("dve", bbX, bbX, tmb, ALU.add)
        pb2 = K.bank(7)
        for cb in range(4):
            K.tr(pb2[:, cb * 128:(cb + 1) * 128], bbX[:, cb * 8:(cb + 1) * 8, :].rearrange("p g h -> p (g h)"), self.ident_f)
        bbT = HA.alloc([128, 4, 128], F32)
        K.cp("act", bbT.rearrange("p c q -> p (c q)"), pb2)
        maskc = HA.alloc([128, 8], F32)
        K.memset("pool", maskc, 1.0)
        K.asel(maskc, maskc, [[-16, 8]], ALU.is_ge, 0.0, 0, 1)
        K.asel(maskc, maskc, [[16, 8]], ALU.is_ge, 0.0, 15, -1)
        BB = HA.alloc([128, 4, 8, 128], BF16)
        K.tt("dve", BB, bbT.unsqueeze(2).to_broadcast([128, 4, 8, 128]),
             maskc.unsqueeze(1).unsqueeze(3).to_broadcast([128, 4, 8, 128]), ALU.mult)
        K.dma("sp", d["BB"], BB.rearrange("p a b c -> p (a b c)"))
        cre_t = HA.alloc([128, 4, 64], F32)
        cim_t = HA.alloc([128, 4, 64], F32)
        K.dma("sp", cre_t, inp["s5_c_re"][0].rearrange("(cb gl) h p -> (gl h) cb p", cb=4))
        K.dma("sp", cim_t, inp["s5_c_im"][0].rearrange("(cb gl) h p -> (gl h) cb p", cb=4))
        CC1 = HA.alloc([128, 4, 128], F32)
        CC2 = HA.alloc([128, 4, 128], F32)
        K.cp("dve", CC1[:, :, 0:64], cre_t)
        K.ts("dve", CC1[:, :, 64:128], cim_t, -1.0, ALU.mult)
        K.ts("dve", CC2[:, :, 0:64], cim_t, -1.0, ALU.mult)
        K.ts("dve", CC2[:, :, 64:128], cre_t, -1.0, ALU.mult)
        mask3 = HA.alloc([128, 8, 128], F32)
        K.memset("pool", mask3, 0.0)
        for a in range(8):
            K.memset("pool", mask3[:, a, a * 16:(a + 1) * 16], 1.0)
        CmX = HA.alloc([128, 4, 128], F32)
        Cp = HA.alloc([128, 4, 8, 128], BF16)
        for CC, nm, bk in ((CC1, "C1", 6), (CC2, "C2", 7)):
            pbc = K.bank(bk)
            for cb in range(4):
                K.tr(pbc[:, cb * 128:(cb + 1) * 128], CC[:, cb, :], self.ident_f)
            K.cp("act", CmX.rearrange("p c q -> p (c q)"), pbc)
            K.tt("dve", Cp, CmX.unsqueeze(2).to_broadcast([128, 4, 8, 128]),
                 mask3.unsqueeze(1).to_broadcast([128, 4, 8, 128]), ALU.mult)
            K.dma("sp", d[nm], Cp.rearrange("p a b c -> p (a b c)"))
        HA.release(hm_small)
        tgi = HA.alloc([128, 128], I32)
        tg = HA.alloc([128, 128], F32)
        K.iota(tgi, [[1, 128]], 0, 0)
        K.cp("dve", tg, tgi)
        ang2 = HA.alloc([128, 8, 128], F32)
        tmp2 = HA.alloc([128, 8, 128], F32)
        sn2 = HA.alloc([128, 8, 128], F32)
        cs2 = HA.alloc([128, 8, 128], F32)
        mg2 = HA.alloc([128, 8, 128], F32)
        A2p = HA.alloc([128, 8, 128], BF16)
        B2p = HA.alloc([128, 8, 128], BF16)
        tgb = tg.unsqueeze(1).to_broadcast([128, 8, 128])
        for cb in range(4):
            gsl = slice(cb * 8, (cb + 1) * 8)
            K.tt("dve", ang2, zi[:, gsl].unsqueeze(2).to_broadcast([128, 8, 128]), tgb, ALU.mult)
            self.sin_rr(sn2, ang2, tmp2)
            K.ts("dve", ang2, ang2, math.pi / 2, ALU.add)
            self.sin_rr(cs2, ang2, tmp2)
            K.tt("dve", mg2, zr[:, gsl].unsqueeze(2).to_broadcast([128, 8, 128]), tgb, ALU.mult)
            K.act(mg2, mg2, AF.Exp)
            K.tt("dve", A2p, mg2, cs2, ALU.mult)
            K.tt("dve", B2p, mg2, sn2, ALU.mult)
            K.dma("sp", d["A2"][:, cb * 1024:(cb + 1) * 1024], A2p.rearrange("p g t -> p (g t)"))
            K.dma("sp", d["B2"][:, cb * 1024:(cb + 1) * 1024], B2p.rearrange("p g t -> p (g t)"))
        HA.release(hm_small)
        zrr = HA.alloc([128, 32, 64], F32)
        zir = HA.alloc([128, 32, 64], F32)
        angT = HA.alloc([128, 2048], F32)
        tmpT = HA.alloc([128, 2048], F32)
        snT = HA.alloc([128, 2048], F32)
        ntc = HA.alloc([128, 1], F32)
        Tb = [HA.alloc([128, 2048], BF16) for _ in range(3)]
        K.dma("sp", zrr.rearrange("p g q -> p (g q)"), a_re.rearrange("g p -> (g p)").partition_broadcast(128))
        K.dma("sp", zir.rearrange("p g q -> p (g q)"), a_im.rearrange("g p -> (g p)").partition_broadcast(128))
        dtb = dt.unsqueeze(2).to_broadcast([128, 32, 64])
        K.tt("dve", zrr, zrr, dtb, ALU.mult)
        K.tt("dve", zir, zir, dtb, ALU.mult)
        zrf = zrr.rearrange("p g q -> p (g q)")
        zif = zir.rearrange("p g q -> p (g q)")
        K.ts("dve", angT, zif, self.pidx, ALU.mult)
        self.sin_rr(snT, angT, tmpT)
        K.ts("dve", angT, angT, math.pi / 2, ALU.add)
        self.sin_rr(zif, angT, tmpT)
        K.ts("dve", ntc, self.pidx, -1.0, ALU.mult)
        K.act(tmpT, zrf, AF.Exp, scale=ntc)
        K.tt("dve", Tb[0], tmpT, zif, ALU.mult)
        K.tt("dve", Tb[2], tmpT, snT, ALU.mult)
        K.ts("dve", Tb[1], Tb[2], -1.0, ALU.mult)
        K.dma("sp", d["TA"], Tb[0])
        K.dma("sp", d["TB"], Tb[1])
        K.dma("sp", d["TBn"], Tb[2])
        HA.release(hm)

    def s5_pass1_begin(self, uT):
        K, HA, NCH = self.K, self.HA, self.NCH
        d = self.s5d
        hm = HA.mark()
        TA = HA.alloc([128, 32, 64], BF16)
        TB = HA.alloc([128, 32, 64], BF16)
        TBn = HA.alloc([128, 32, 64], BF16)
        BB = HA.alloc([128, 4, 8, 128], BF16)
        Lc = HA.alloc([128, 6, 32], F32)
        K.dma("sp", TA.rearrange("p g q -> p (g q)"), d["TA"])
        K.dma("sp", TB.rearrange("p g q -> p (g q)"), d["TB"])
        K.dma("sp", TBn.rearrange("p g q -> p (g q)"), d["TBn"])
        K.dma("sp", BB.rearrange("p a b c -> p (a b c)"), d["BB"])
        K.dma("sp", Lc.rearrange("p a g -> p (a g)"), d["L"])
        PA = [HA.alloc([128, 4, 8, 2, 64], BF16) for _ in range(2)]
        PB = [HA.alloc([128, 4, 8, 2, 64], BF16) for _ in range(2)]
        self.pa_d = self.dint("s5_pa", [NCH, 128, 4096], BF16)
        self.pb_d = self.dint("s5_pb", [NCH, 128, 4096], BF16)
        X = HA.alloc([128, 32], F32)
        Zs = HA.alloc([128, 32], F32)
        t1, t2, tA, tB = (HA.alloc([128, 32], F32) for _ in range(4))
        K.memset("pool", X, 0.0)
        flat5 = "p a b c d -> p (a b c d)"

        def step(n):
            csl = slice(n * 128, (n + 1) * 128)
            pa, pb = PA[n % 2], PB[n % 2]
            for cb in range(4):
                pbu = self.K.psum[:, 6 * 512:8 * 512]
                K.mm(pbu[:, 0:512], uT[:, cb, csl], BB[:, cb, 0:4, :].rearrange("p g q -> p (g q)"))
                K.mm(pbu[:, 512:1024], uT[:, cb, csl], BB[:, cb, 4:8, :].rearrange("p g q -> p (g q)"))
                bu4 = pbu.rearrange("p (g r q) -> p g r q", g=8, r=2)
                gs = slice(cb * 8, (cb + 1) * 8)
                K.tt("dve", pa[:, cb], bu4, TA[:, gs, :].unsqueeze(2).to_broadcast([128, 8, 2, 64]), ALU.mult)
                K.tt("dve", pb[:, cb, :, 0, :], bu4[:, :, 1, :], TBn[:, gs, :], ALU.mult)
                K.tt("dve", pb[:, cb, :, 1, :], bu4[:, :, 0, :], TB[:, gs, :], ALU.mult)
            K.dma("sp", self.pa_d[n], pa.rearrange(flat5))
            K.dma("sp", self.pb_d[n], pb.rearrange(flat5))
            pz = K.bank(5)[:, 0:32]
            for g in range(32):
                cb, gl = divmod(g, 8)
                K.mm(pz[:, g:g + 1], pa[:, cb, gl].rearrange("p r q -> p (r q)"), self.ones_b[:, 0:1], True, False)
                K.mm(pz[:, g:g + 1], pb[:, cb, gl].rearrange("p r q -> p (r q)"), self.ones_b[:, 0:1], False, True)
            K.cp("act", Zs, pz)
            self.cmul(t1, Lc[:, 2, :], Lc[:, 3, :], Zs, tA, tB)
            self.cmul(t2, Lc[:, 4, :], Lc[:, 5, :], X, tA, tB)
            K.tt("dve", X, t1, t2, ALU.add)

        def finish(st_loc):
            K.dma("sp", st_loc[:, 512:544], X)
            HA.release(hm)

        return step, finish

    def s5_pass2(self, uT, st_all, mergedT):
        K, A, HA, NCH = self.K, self.A, self.HA, self.NCH
        d = self.s5d
        inp = self.inp
        hm = HA.mark()
        A2 = HA.alloc([128, 32, 128], BF16)
        B2 = HA.alloc([128, 32, 128], BF16)
        C1 = HA.alloc([128, 32, 128], BF16)
        C2 = HA.alloc([128, 32, 128], BF16)
        for t_, nm in ((A2, "A2"), (B2, "B2"), (C1, "C1"), (C2, "C2")):
            K.dma("sp", t_.rearrange("p g t -> p (g t)"), d[nm])
        XA = HA.alloc([128, 32, 128], BF16)
        XB = HA.alloc([128, 32, 128], BF16)
        Eall = HA.alloc([128, 32, 128], BF16)
        K.memset("pool", Eall, 1.0)
        K.asel(Eall, Eall, [[-1, 32], [0, 128]], ALU.is_equal, 0.0, 0, 1)
        PA = [A.alloc([128, 4, 8, 2, 64], BF16) for _ in range(2)]
        PB = [A.alloc([128, 4, 8, 2, 64], BF16) for _ in range(2)]
        Lc = A.alloc([128, 6, 32], F32)
        K.dma("sp", Lc.rearrange("p a g -> p (a g)"), d["L"])
        Tri = A.alloc([128, 128], BF16)
        K.memset("pool", Tri, 1.0)
        K.asel(Tri, Tri, [[1, 128]], ALU.is_ge, 0.0, 0, -1)
        wglu = A.alloc([128, 4, 512], BF16)
        self.wload(wglu, inp["s5_w_glu"][0].rearrange("(k p) n -> p k n", p=128), (4, 512))
        dcol = A.alloc([128, 4], F32)
        bglu = A.alloc([128, 4], F32)
        K.dma("sp", dcol, inp["s5_d"][0].rearrange("(cb gl) h -> (gl h) cb", cb=4), allow_slow_non_contiguous=True)
        K.dma("sp", bglu, inp["s5_b_glu"][0].rearrange("(cb p) -> p cb", cb=4), allow_slow_non_contiguous=True)
        X = A.alloc([128, 32], F32)
        Xin = A.alloc([128, 32], F32)
        Xc = A.alloc([128, 32], F32)
        Zc = A.alloc([128, 32], F32)
        tA = A.alloc([128, 32], F32)
        tB = A.alloc([128, 32], F32)
        XinT = A.alloc([128, 128], BF16)
        K.memset("pool", XinT, 0.0)
        yv = A.alloc([128, 4, 128], F32)
        y2 = A.alloc([128, 512], F32)
        sgm = A.alloc([128, 512], F32)
        gT = A.alloc([128, 4, 128], BF16)
        sig2 = A.alloc([128, 4, 128], F32)
        K.dma("sp", Xin, st_all[0:128, 512:544])
        K.ts("dve", X, Xin, self.flag, ALU.mult)
        flat5 = "p a b c d -> p (a b c d)"
        yvf = yv.rearrange("p c t -> p (c t)")
        for n in range(NCH):
            csl = slice(n * 128, (n + 1) * 128)
            pa, pb = PA[n % 2], PB[n % 2]
            K.dma("sp", pa.rearrange(flat5), self.pa_d[n])
            K.dma("sp", pb.rearrange(flat5), self.pb_d[n])
            self.cmul(Xc, Lc[:, 0, :], Lc[:, 1, :], X, tA, tB)
            pc = K.bank(3)
            K.tr(pc[0:32, 0:128], Xc, self.ident_f)
            K.cp("act", XinT[0:32], pc[0:32, 0:128])
            for gb in range(8):
                pz = K.bank(6 + gb % 2)
                for gi in range(4):
                    g = gb * 4 + gi
                    cb, gl = divmod(g, 8)
                    o_ = pz[:, gi * 128:(gi + 1) * 128]
                    K.mm(o_, pa[:, cb, gl].rearrange("p r q -> p (r q)"), Tri, True, False)
                    K.mm(o_, pb[:, cb, gl].rearrange("p r q -> p (r q)"), Tri, False, False)
                    K.mm(o_, XinT, Eall[:, g, :], False, True)
                gsl = slice(gb * 4, (gb + 1) * 4)
                K.tt("dve", XA[:, gsl, :].rearrange("p g t -> p (g t)"), pz, A2[:, gsl, :].rearrange("p g t -> p (g t)"), ALU.mult)
                K.tt("dve", XB[:, gsl, :].rearrange("p g t -> p (g t)"), pz, B2[:, gsl, :].rearrange("p g t -> p (g t)"), ALU.mult)
                K.cp("act", Zc[:, gsl], pz.rearrange("p (g t) -> p g t", g=4)[:, :, 127])
            self.cmul(X, Lc[:, 2, :], Lc[:, 3, :], Zc, tA, tB)
            py = K.bank(5)
            for cb in range(4):
                for gl in range(8):
                    g = cb * 8 + gl
                    K.mm(py[:, cb * 128:(cb + 1) * 128], C1[:, g, :], XA[:, g, :], gl == 0, False)
                    K.mm(py[:, cb * 128:(cb + 1) * 128], C2[:, g, :], XB[:, g, :], False, gl == 7)
            for cb in range(4):
                K.stt(yv[:, cb, :], uT[:, cb, csl], dcol[:, cb:cb + 1], py[:, cb * 128:(cb + 1) * 128], ALU.mult, ALU.add)
            K.tt("pool", y2, yvf, yvf, ALU.mult)
            K.ts("pool", y2, y2, 0.044715, ALU.mult, 1.0, ALU.add)
            K.tt("pool", y2, y2, yvf, ALU.mult)
            K.act(sgm, y2, AF.Sigmoid, scale=1.5957691216057308)
            K.tt("dve", gT.rearrange("p c t -> p (c t)"), yvf, sgm, ALU.mult)
            pzg = K.bank(4)
            for co in range(4):
                for ci in range(4):
                    K.mm(pzg[:, co * 128:(co + 1) * 128], wglu[:, ci, co * 128:(co + 1) * 128], gT[:, ci, :], ci == 0, ci == 3)
            for co in range(4):
                K.act(sig2[:, co, :], pzg[:, co * 128:(co + 1) * 128], AF.Sigmoid, bias=bglu[:, co:co + 1])
            K.tt("dve", mergedT[:, 4:8, csl], gT, sig2, ALU.mult)
        HA.release(hm)

    def moe(self, l):
        K, A, NCH, T, NTB = self.K, self.A, self.NCH, self.T, self.NTB
        inp = self.inp
        h, xnT = self.h, self.xnT
        m0 = A.mark()
        wg = [A.alloc([128, KC, 512], BF16) for _ in range(2)]
        wu = [A.alloc([128, KC, 512], BF16) for _ in range(2)]
        wd = [A.alloc([128, 4, D], BF16) for _ in range(2)]
        cw = A.alloc([128, NCH, 16], F32)
        wr32 = A.alloc([128, KC, 20], F32)
        b20 = A.alloc([128, 20], F32)
        xn32 = A.alloc([128, D], F32)
        xnb = A.alloc([128, D], BF16)
        xT32 = xn32.rearrange("p (k t) -> p k t", k=KC)
        rts = [A.alloc([128, 4, 64], F32) for _ in range(2)]
        hT = [A.alloc([128, 4, 512], BF16) for _ in range(2)]
        sgt = [A.alloc([128, 512], F32) for _ in range(2)]
        self.load_gamma(inp["ffn_norm"][l])
        K.dma("sp", wr32[:, :, 0:4], inp["moe_w_group"][l].rearrange("(k p) g -> p k g", p=128))
        K.dma("sp", wr32[:, :, 4:20], inp["moe_w_router"][l].rearrange("(k p) g e -> p k (g e)", p=128))
        K.dma("sp", b20[:, 0:4], inp["moe_b_group"][l].partition_broadcast(128))
        K.dma("sp", b20[:, 4:20], inp["moe_b_router"][l].rearrange("g e -> (g e)").partition_broadcast(128))

        def wview_in(w):
            return w.rearrange("(k p) f -> p k f", p=128)

        def load_expert(e, s):
            g_, e_ = divmod(e, 4)
            vg = wview_in(inp["moe_w_gate"][l, g_, e_])
            vu = wview_in(inp["moe_w_up"][l, g_, e_])
            vd = inp["moe_w_down"][l, g_, e_].rearrange("(c p) d -> p c d", p=128)
            for j in range(2):
                self.wload(wg[s][:, 4 * j:4 * j + 4, :], vg[:, 4 * j:4 * j + 4, :], (4, 512))
            for j in range(2):
                self.wload(wu[s][:, 4 * j:4 * j + 4, :], vu[:, 4 * j:4 * j + 4, :], (4, 512))
            for j in range(2):
                self.wload(wd[s][:, 2 * j:2 * j + 2, :], vd[:, 2 * j:2 * j + 2, :], (2, D))

        load_expert(0, 0)

        def bc(ap, shape):
            return ap.to_broadcast(shape)

        def routing4(c0, pl4, rt):
            S4 = [128, 4, 4]
            L = rt[:, :, 0:20]
            gm, ohg, dg, sumg, gval = rt[:, :, 20], rt[:, :, 21:25], rt[:, :, 25:29], rt[:, :, 29], rt[:, :, 30]
            es, mx1, oh1, es2, mx2, oh2 = rt[:, :, 32:36], rt[:, :, 36], rt[:, :, 37:41], rt[:, :, 41:45], rt[:, :, 45], rt[:, :, 46:50]
            dd, ed, w1, w2, win, tmp = rt[:, :, 50], rt[:, :, 51], rt[:, :, 52], rt[:, :, 53], rt[:, :, 54:58], rt[:, :, 58:62]

            def rmax(out, in_):
                K.S.add("dve", lambda e: e.tensor_reduce(out=out, in_=in_, axis=AX.X, op=ALU.max), [in_], [out])

            K.tt("dve", L, pl4, bc(b20.unsqueeze(1), [128, 4, 20]), ALU.add)
            rmax(gm, L[:, :, 0:4])
            K.tt("dve", ohg, L[:, :, 0:4], bc(gm.unsqueeze(2), S4), ALU.is_equal)
            K.tt("dve", dg, L[:, :, 0:4], bc(gm.unsqueeze(2), S4), ALU.subtract)
            K.act(dg, dg, AF.Exp)
            K.S.add("dve", lambda e: e.tensor_reduce(out=sumg, in_=dg, axis=AX.X, op=ALU.add), [dg], [sumg])
            K.recip(gval, sumg)
            K.tt("dve", es, L[:, :, 4:8], bc(ohg[:, :, 0:1], S4), ALU.mult)
            for g_ in range(1, 4):
                K.tt("dve", tmp, L[:, :, 4 + 4 * g_:8 + 4 * g_], bc(ohg[:, :, g_:g_ + 1], S4), ALU.mult)
                K.tt("dve", es, es, tmp, ALU.add)
            rmax(mx1, es)
            K.tt("dve", oh1, es, bc(mx1.unsqueeze(2), S4), ALU.is_equal)
            K.stt(es2, oh1, -1e30, es, ALU.mult, ALU.add)
            rmax(mx2, es2)
            K.tt("dve", oh2, es2, bc(mx2.unsqueeze(2), S4), ALU.is_equal)
            K.tt("dve", dd, mx2, mx1, ALU.subtract)
            K.act(ed, dd, AF.Exp)
            K.ts("dve", w1, ed, 1.0, ALU.add)
            K.recip(w1, w1)
            K.tt("dve", w2, ed, w1, ALU.mult)
            K.tt("dve", w1, w1, gval, ALU.mult)
            K.tt("dve", w2, w2, gval, ALU.mult)
            K.tt("dve", win, oh1, bc(w1.unsqueeze(2), S4), ALU.mult)
            K.tt("dve", tmp, oh2, bc(w2.unsqueeze(2), S4), ALU.mult)
            K.tt("dve", win, win, tmp, ALU.add)
            for g_ in range(4):
                K.tt("dve", cw[:, c0:c0 + 4, 4 * g_:4 * g_ + 4], win, bc(ohg[:, :, g_:g_ + 1], S4), ALU.mult)

        def prepass(c):
            ss = self.stat_slot()
            src = h[:, c, :]
            K.act(xnb, src, AF.Square, accum=ss[:, 0:1])
            K.ts("dve", ss[:, 1:2], ss[:, 0:1], 1.0 / D, ALU.mult, 1e-6, ALU.add)
            K.act(ss[:, 2:3], ss[:, 1:2], AF.Sqrt)
            K.recip(ss[:, 3:4], ss[:, 2:3])
            K.stt(xnb, src, ss[:, 3:4], self.gB, ALU.mult, ALU.mult)
            K.stt(xn32, src, ss[:, 3:4], self.gB, ALU.mult, ALU.mult)
            self.to_xnT(xnb, c, c % 2)
            p32 = K.psum[:, 6 * 512:8 * 512]
            for k in range(KC):
                K.tr(p32[:, k * 128:(k + 1) * 128], xn32[:, k * 128:(k + 1) * 128], self.ident_f)
            K.cp("act", xn32, p32)
            tbi, cc = divmod(c, 4)
            pl4 = K.bank(4 + tbi % 2)[:, 0:80]
            for k in range(KC):
                K.mm(pl4[:, cc * 20:(cc + 1) * 20], xT32[:, k, :], wr32[:, k, :], k == 0, k == KC - 1)
            if cc == 3:
                routing4(c - 3, pl4.rearrange("p (c j) -> p c j", c=4), rts[tbi % 2])

        it = 0
        io = 0
        for e in range(16):
            s = e % 2
            if e + 1 < 16:
                load_expert(e + 1, (e + 1) % 2)
            for tb in range(NTB):
                if e == 0:
                    for c in range(tb * 4, tb * 4 + 4):
                        prepass(c)
                tsl = slice(tb * 512, (tb + 1) * 512)
                hTb = hT[(e * NTB + tb) % 2]
                for fb in range(4):
                    pg = K.bank(it % 2)
                    pu = K.bank(2 + it % 2)
                    fsl = slice(fb * 128, (fb + 1) * 128)
                    for kc in range(KC):
                        K.mm(pg, wg[s][:, kc, fsl], xnT[:, kc, tsl], kc == 0, kc == KC - 1)
                    for kc in range(KC):
                        K.mm(pu, wu[s][:, kc, fsl], xnT[:, kc, tsl], kc == 0, kc == KC - 1)
                    K.act(sgt[it % 2], pg, AF.Silu)
                    K.tt("dve", hTb[:, fb, :], sgt[it % 2], pu, ALU.mult)
                    it += 1
                for cc in range(4):
                    c = tb * 4 + cc
                    for dh in range(2):
                        po = K.bank(4 + io % 4)
                        io += 1
                        dsl = slice(dh * 512, (dh + 1) * 512)
                        for fc in range(4):
                            K.mm(po, hTb[:, fc, cc * 128:(cc + 1) * 128], wd[s][:, fc, dsl], fc == 0, fc == 3)
                        K.stt(h[:, c, dsl], po, cw[:, c, e:e + 1], h[:, c, dsl], ALU.mult, ALU.add)
        A.release(m0)

    def l1mix(self):
        K, A, NCH, T = self.K, self.A, self.NCH, self.T
        assert NCH == 16
        inp = self.inp
        h, xnT = self.h, self.xnT
        m0 = A.mark()
        xnb = [A.alloc([128, D], BF16) for _ in range(2)]
        junk = A.alloc([128, D], BF16)
        self.load_gamma(inp["mix_norm"][1])
        for c in range(NCH):
            self.rms(h[:, c, :], xnb[c % 2], junk)
            self.to_xnT(xnb[c % 2], c, c % 2)
        A.release(m0)
        Wqkv = inp["c_w_qkv"][0].rearrange("(k p) n -> p k n", p=128)
        qT = A.alloc([128, 8, T], BF16)
        m1 = A.mark()
        wgrp = [A.alloc([128, KC, 512], BF16) for _ in range(2)]
        stg = [A.alloc([128, T], BF16) for _ in range(2)]
        kv_loc = [self.dint("kv_loc%d" % g, [4 * 128, T], BF16) for g in range(4)]
        kv_all = [self.dint("kv_all%d" % g, [2 * 4 * 128, T], BF16) for g in range(4)]
        it = 0
        gorder = [2, 3, 4, 5, 0, 1]

        def load_qkv_group(gi):
            g2 = gorder[gi]
            for j in range(2):
                self.wload(wgrp[gi % 2][:, 4 * j:4 * j + 4, :], Wqkv[:, 4 * j:4 * j + 4, g2 * 512:(g2 + 1) * 512], (4, 512))

        load_qkv_group(0)
        for bi in range(24):
            gi, hb = divmod(bi, 4)
            grp = gorder[gi]
            blk = grp * 4 + hb
            if hb == 0 and gi + 1 < 6:
                load_qkv_group(gi + 1)
            wb = wgrp[gi % 2][:, :, hb * 128:(hb + 1) * 128]
            for tb in range(4):
                ps = K.bank(2 + it % 4)
                it += 1
                tsl = slice(tb * 512, (tb + 1) * 512)
                for kc in range(KC):
                    K.mm(ps, wb[:, kc, :], xnT[:, kc, tsl], kc == 0, kc == KC - 1)
                if blk < 8:
                    K.act(qT[:, blk, tsl], ps, AF.Copy, scale=float(128.0 ** -0.5))
                elif tb % 2 == 0:
                    K.cp("act", stg[blk % 2][:, tsl], ps)
                else:
                    K.cp("dve", stg[blk % 2][:, tsl], ps)
            if blk >= 8:
                g4, j4 = divmod(blk - 8, 4)
                K.dma("sp", kv_loc[g4][j4 * 128:(j4 + 1) * 128, :], stg[blk % 2])
                if j4 == 3:
                    K.S.add("pool", lambda e, g4=g4: e.collective_compute(
                        "AllGather", ALU.bypass, replica_groups=self.rg,
                        ins=[kv_loc[g4].opt()], outs=[kv_all[g4].opt()]),
                        [kv_loc[g4]], [kv_all[g4]], dma="cc")
        A.release(m1)
        oT = xnT
        KTb = [A.alloc([128, 2 * T], BF16), self.stage[0].bitcast(BF16)]
        VTb = [A.alloc([128, 2 * T], BF16), self.stage[1].bitcast(BF16)]
        sqb = [A.alloc([128, 512], BF16) for _ in range(2)]
        maskOP = A.alloc([128, 2, 128], BF16)
        NR = 4
        Vd = [A.alloc([128, 128], BF16) for _ in range(NR)]
        PT = [A.alloc([128, 256], BF16) for _ in range(NR)]
        Oa = A.alloc([128, 1024], F32)
        La = A.alloc([128, 1024], F32)
        rl = A.alloc([128, 1024], F32)
        sm = A.alloc([128, 16], F32)
        cb = A.alloc([128, 2], BF16)
        cneg = A.alloc([128, 2], F32)
        K.memset("pool", maskOP, 1.0)
        K.asel(maskOP[:, 0, :], maskOP[:, 0, :], [[1, 128]], ALU.is_ge, 0.0, 0, -1)
        K.asel(maskOP[:, 1, :], maskOP[:, 1, :], [[-1, 128]], ALU.is_ge, 0.0, 0, 1)
        onec = self.ones_b[:, 0:1]

        def prologue(hd):
            KT, VT = KTb[hd % 2], VTb[hd % 2]
            gk, gv, j4 = hd // 4, 2 + hd // 4, hd % 4
            K.dma("sp", KT[:, 0:T], kv_all[gk][j4 * 128:(j4 + 1) * 128, :])
            K.dma("sp", KT[:, T:2 * T], kv_loc[gk][j4 * 128:(j4 + 1) * 128, :])
            K.dma("sp", VT[:, 0:T], kv_all[gv][j4 * 128:(j4 + 1) * 128, :])
            K.dma("sp", VT[:, T:2 * T], kv_loc[gv][j4 * 128:(j4 + 1) * 128, :])
            pn = K.bank(7)
            for j in range(12):
                sq = sqb[j % 2]
                src = KT[:, j * 512:(j + 1) * 512] if j < 8 else qT[:, hd, (j - 8) * 512:(j - 7) * 512]
                K.tt("pool", sq, src, src, ALU.mult)
                K.mm(pn[0:1, :], onec, sq)
                K.S.add("dve", lambda e, j=j: e.tensor_reduce(out=sm[0:1, j:j + 1], in_=pn[0:1, :], axis=AX.X, op=ALU.max),
                        [pn[0:1, :]], [sm[0:1, j:j + 1]])
            K.S.add("dve", lambda e: e.tensor_reduce(out=sm[0:1, 12:13], in_=sm[0:1, 0:8], axis=AX.X, op=ALU.max),
                    [sm[0:1, 0:8]], [sm[0:1, 12:13]])
            K.S.add("dve", lambda e: e.tensor_reduce(out=sm[0:1, 13:14], in_=sm[0:1, 8:12], axis=AX.X, op=ALU.max),
                    [sm[0:1, 8:12]], [sm[0:1, 13:14]])
            K.tt("dve", sm[0:1, 14:15], sm[0:1, 12:13], sm[0:1, 13:14], ALU.mult)
            K.act(cb[0:1, hd % 2:hd % 2 + 1], sm[0:1, 14:15], AF.Sqrt)
            K.mm(pn[:, 0:1], self.ones_b[0:1, :], cb[0:1, hd % 2:hd % 2 + 1])
            K.act(cneg[:, hd % 2:hd % 2 + 1], pn[:, 0:1], AF.Copy, scale=-1.0)

        units = []
        for hd in range(8):
            for qh in range(2):
                sub1 = []
                for kb in range(8 * qh - 1, 8 * qh + 8):
                    qb = [b_ for b_ in (kb, kb + 1) if 8 * qh <= b_ < 8 * qh + 8]
                    kinds = [0 if b_ == kb else 1 for b_ in qb]
                    u = dict(hd=hd, qh=qh, ks=ssl(T + 128 * kb, 128, 1), qs=ssl(128 * qb[0], 128 * len(qb), 1),
                             NQ=128 * len(qb), kinds=kinds, j0=0, flg=(kb == -1), pv=[])
                    for i_, b_ in enumerate(qb):
                        bl = b_ - 8 * qh
                        u["pv"].append([bl // 4, slice((bl % 4) * 128, (bl % 4) * 128 + 128), slice(128 * i_, 128 * i_ + 128)])
                    sub1.append(u)
                for r in range(4):
                    for kb in range(2 * qh - 1, 2 * qh + 2):
                        qb = [b_ for b_ in (kb, kb + 1) if 2 * qh <= b_ < 2 * qh + 2]
                        kinds = [0 if b_ == kb else 1 for b_ in qb]
                        u = dict(hd=hd, qh=qh, ks=ssl(T + 512 * kb + r, 128, 4), qs=ssl(512 * qb[0] + r, 128 * len(qb), 4),
                                 NQ=128 * len(qb), kinds=kinds, j0=0, flg=(kb == -1), pv=[])
                        for i_, b_ in enumerate(qb):
                            u["pv"].append([b_ - 2 * qh, ssl(r, 128, 4), slice(128 * i_, 128 * i_ + 128)])
                        sub1.append(u)
                sub2 = []
                for r in range(16):
                    for kind in (1, 0):
                        k0 = r if kind == 1 else T + r
                        u = dict(hd=hd, qh=qh, ks=ssl(k0, 128, 16), qs=ssl(16 * 64 * qh + r, 64, 16), NQ=64,
                                 kinds=[kind], j0=64 * qh, flg=(kind == 1),
                                 pv=[[r // 8, slice((r % 8) * 64, (r % 8) * 64 + 64), slice(0, 64)]])
                        sub2.append(u)
                for sub, endk in ((sub1, "evac"), (sub2, "final")):
                    first, last = {}, {}
                    for ui, u in enumerate(sub):
                        for pi, p_ in enumerate(u["pv"]):
                            first.setdefault(p_[0], (ui, pi))
                            last[p_[0]] = (ui, pi)
                    for ui, u in enumerate(sub):
                        for pi, p_ in enumerate(u["pv"]):
                            p_.append(first[p_[0]] == (ui, pi))
                            p_.append(last[p_[0]] == (ui, pi))
                        u["end"] = endk if ui == len(sub) - 1 else None
                    units.extend(sub)

        def stageA_pe(idx):
            u = units[idx]
            hd = u["hd"]
            KT, VT = KTb[hd % 2], VTb[hd % 2]
            pvt = K.bank(7, 128, BF16)
            K.tr(pvt, VT[:, u["ks"]], self.ident_b)
            ps = K.bank(4 + idx % 3)
            K.mm(ps[:, 0:u["NQ"]], KT[:, u["ks"]], qT[:, hd, u["qs"]], True, True)

        def stageA_cp(idx):
            pvt = K.bank(7, 128, BF16)
            K.cp("act" if idx % 2 == 0 else "dve", Vd[idx % NR], pvt)

        def stageB(idx):
            u = units[idx]
            hd, NQ, j0 = u["hd"], u["NQ"], u["j0"]
            ps = K.bank(4 + idx % 3)
            ptv = PT[idx % NR][:, 0:NQ]
            K.act(ptv, ps[:, 0:NQ], AF.Exp, bias=cneg[:, hd % 2:hd % 2 + 1])
            if len(u["kinds"]) == 2:
                K.tt("dve", ptv.rearrange("p (a q) -> p a q", a=2), ptv.rearrange("p (a q) -> p a q", a=2), maskOP, ALU.mult)
            else:
                mk = maskOP[:, u["kinds"][0], j0:j0 + NQ]
                if u["flg"]:
                    K.stt(ptv, ptv, self.flag, mk, ALU.mult, ALU.mult)
                else:
                    K.tt("dve", ptv, ptv, mk, ALU.mult)

        def stageC(idx):
            u = units[idx]
            hd, qh = u["hd"], u["qh"]
            vd = Vd[idx % NR]
            pt = PT[idx % NR]
            for (b, osl, psl, st_, sp_) in u["pv"]:
                K.mm(K.bank(b)[:, osl], vd, pt[:, psl], st_, sp_)
                K.mm(K.bank(2 + b)[:, osl], self.ones_b, pt[:, psl], st_, sp_)
            Oacc = K.psum[:, 0:2 * 512]
            Lacc = K.psum[:, 2 * 512:4 * 512]
            if u["end"] == "evac":
                K.cp("act", Oa, Oacc)
                K.cp("dve", La, Lacc)
            elif u["end"] == "final":
                nat = "p (r j) -> p j r"
                K.tt("dve", La.rearrange("p (j r) -> p j r", r=16), La.rearrange("p (j r) -> p j r", r=16),
                     Lacc.rearrange(nat, r=16), ALU.add)
                K.act(rl, La, AF.Ln)
                K.act(rl, rl, AF.Exp, scale=-1.0)
                K.tt("dve", Oa.rearrange("p (j r) -> p j r", r=16), Oa.rearrange("p (j r) -> p j r", r=16),
                     Oacc.rearrange(nat, r=16), ALU.add)
                K.tt("dve", oT[:, hd, qh * 1024:(qh + 1) * 1024], Oa, rl, ALU.mult)

        NU = len(units)
        prologue(0)
        prologue(1)
        stageA_pe(0)
        stageA_cp(0)
        stageA_pe(1)
        stageA_cp(1)
        stageB(0)
        for idx in range(NU):
            u = units[idx]
            if idx > 0 and units[idx - 1]["hd"] != u["hd"] and u["hd"] + 1 < 8:
                prologue(u["hd"] + 1)
            if idx + 2 < NU:
                stageA_pe(idx + 2)
            if idx + 1 < NU:
                stageB(idx + 1)
            if idx + 2 < NU:
                stageA_cp(idx + 2)
            stageC(idx)
        self.dump("oT", oT, [8, 128, T], "h p t -> p h t")
        A.release(m1)
        wo = A.alloc([128, 8, D], BF16)
        Wout = inp["c_w_out"][0].rearrange("(k p) n -> p k n", p=128)
        for j in range(4):
            self.wload(wo[:, :, j * 256:(j + 1) * 256], Wout[:, :, j * 256:(j + 1) * 256], (KC, 256))
        for c in range(NCH):
            csl = slice(c * 128, (c + 1) * 128)
            for dh in range(2):
                po = K.bank(6 + dh)
                dsl = slice(dh * 512, (dh + 1) * 512)
                for hd in range(8):
                    K.mm(po, oT[:, hd, csl], wo[:, hd, dsl], hd == 0, hd == 7)
                K.tt("dve", h[:, c, dsl], po, h[:, c, dsl], ALU.add)
        A.release(m0)


FULL_STAGES = ("l0mix", "s5", "moe0", "l1mix", "moe1", "final")


def kernel(**inputs):
    inp = {k: np.ascontiguousarray(np.asarray(v, dtype=np.float32)) for k, v in inputs.items()}
    x = inp["x"]
    B, L, _ = x.shape
    assert (B, L) == (4, 4096)
    P = Prog(NCH=16, stages=FULL_STAGES, n_cores=NCORES)
    shared = {k: v for k, v in inp.items() if k != "x"}
    maps = []
    for c in range(NCORES):
        b, half = divmod(c, 2)
        m = dict(shared)
        m["x"] = np.ascontiguousarray(x[b, half * 2048:(half + 1) * 2048])
        m["flag"] = np.full((128, 1), float(half), np.float32)
        maps.append(m)
    res = run_bass_kernel_spmd(P.nc, maps, core_ids=list(range(NCORES)))
    out = np.empty((B, L, D), np.float32)
    for c in range(NCORES):
        b, half = divmod(c, 2)
        out[b, half * 2048:(half + 1) * 2048] = np.asarray(res.results[c]["y"], dtype=np.float32)
    return out
```

```python
import math
import os
import numpy as np
from contextlib import ExitStack
import concourse.bass as bass
import concourse.mybir as mybir
from concourse.bass_utils import run_bass_kernel_spmd

F32 = mybir.dt.float32
BF16 = mybir.dt.bfloat16
I32 = mybir.dt.int32
AF = mybir.ActivationFunctionType
ALU = mybir.AluOpType
AX = mybir.AxisListType

D = 1024
KC = 8
NCORES = 8
MAGIC = 12582912.0
TWO_PI = 2.0 * math.pi


def ssl(start, n, step=1):
    return slice(start, start + (n - 1) * step + 1, step)


def _isap(x):
    return isinstance(x, bass.AP)


def _region(ap):
    name = ap.tensor.name
    pat = ap.ap
    sz = mybir.dt.size(ap.dtype)
    off = ap.offset
    if str(ap.space) == "DRAM":
        lo = off
        hi = off
        for (s, c) in pat:
            d = (c - 1) * s
            if d < 0:
                lo += d
            else:
                hi += d
        return (name, 0, 1, lo * sz, (hi + 1) * sz)
    pstep, pcnt = pat[0]
    if pstep == 0:
        p0 = 0
        f = off
        pcnt = 128
    else:
        p0 = off // pstep
        f = off - p0 * pstep
    lo = f
    hi = f
    for (s, c) in pat[1:]:
        d = (c - 1) * s
        if d < 0:
            lo += d
        else:
            hi += d
    lo_b = lo * sz
    hi_b = (hi + 1) * sz
    if str(ap.space) == "PSUM":
        return (name, 0, 128, (lo_b // 2048) * 2048, ((hi_b + 2047) // 2048) * 2048)
    return (name, p0, p0 + pcnt, lo_b, hi_b)


class Op:
    __slots__ = ("id", "eng", "fn", "deps", "is_dma", "signal", "idx", "sem", "semval")


class Sched:
    ENGS = ("pe", "act", "dve", "pool", "sp")

    def __init__(self, nc, n_dma_sems=8):
        self.nc = nc
        self.ops = []
        self.by_eng = {e: [] for e in self.ENGS}
        self.recs = {}
        self.n_dma_sems = n_dma_sems
        self.dma_rr = {e: 0 for e in self.ENGS}
        self.dma_rr["cc"] = 0
        self.dma_last = {}

    def _touch(self, op, aps, is_write):
        deps = op.deps
        for ap in aps:
            if ap is None or not _isap(ap):
                continue
            name, p0, p1, f0, f1 = _region(ap)
            lst = self.recs.get(name, [])
            keep = []
            for r in lst:
                rp0, rp1, rf0, rf1, rid, rw, reng = r
                ov = not (rp1 <= p0 or p1 <= rp0 or rf1 <= f0 or f1 <= rf0)
                if ov and (is_write or rw) and rid != op.id:
                    deps.add(rid)
                if is_write and rp0 >= p0 and rp1 <= p1 and rf0 >= f0 and rf1 <= f1 and rid != op.id:
                    continue
                if (not is_write) and (not rw) and reng == op.eng and (not op.is_dma) \
                        and (not self.ops[rid].is_dma) and (rp0, rp1, rf0, rf1) == (p0, p1, f0, f1):
                    continue
                keep.append(r)
            keep.append((p0, p1, f0, f1, op.id, is_write, op.eng))
            self.recs[name] = keep

    def add(self, eng, fn, reads=(), writes=(), dma=False):
        op = Op()
        op.id = len(self.ops)
        op.eng = eng
        op.fn = fn
        op.deps = set()
        op.is_dma = bool(dma)
        op.signal = bool(dma)
        op.sem = None
        op.semval = None
        self.ops.append(op)
        self._touch(op, reads, False)
        self._touch(op, writes, True)
        if dma == "cc":
            op.sem = ("cc", self.dma_rr["cc"])
            self.dma_rr["cc"] += 1
            op.semval = 1
            self.dma_last[op.sem] = op
        elif dma:
            slot = (eng, self.dma_rr[eng] % self.n_dma_sems)
            self.dma_rr[eng] += 1
            prev = self.dma_last.get(slot)
            if prev is not None:
                op.deps.add(prev.id)
                op.semval = prev.semval + 16
            else:
                op.semval = 16
            op.sem = slot
            self.dma_last[slot] = op
        op.idx = len(self.by_eng[eng])
        self.by_eng[eng].append(op)
        return op

    def plan(self):
        ops = self.ops
        for op in ops:
            for d in op.deps:
                dop = ops[d]
                if dop.is_dma:
                    continue
                if dop.eng == "pe" and op.eng == "pe" and not op.is_dma:
                    continue
                dop.signal = True
        for e in self.ENGS:
            c = 0
            for op in self.by_eng[e]:
                if (not op.is_dma) and op.signal:
                    c += 1
                    op.semval = c
        know = {e: {f: 0 for f in self.ENGS} for e in self.ENGS}
        know_dma = {e: set() for e in self.ENGS}
        snap = {}
        waits = {}
        for op in ops:
            e = op.eng
            w = []
            need = {}
            for d in sorted(op.deps):
                dop = ops[d]
                if dop.is_dma:
                    if d not in know_dma[e]:
                        w.append(("dma", dop.sem, dop.semval))
                        know_dma[e].add(d)
                    continue
                if dop.eng == "pe" and e == "pe" and not op.is_dma:
                    continue
                if know[e][dop.eng] >= dop.semval:
                    continue
                need[dop.eng] = max(need.get(dop.eng, 0), dop.semval)
            for f, v in sorted(need.items(), key=lambda kv: -kv[1]):
                if know[e][f] >= v:
                    continue
                w.append(("eng", f, v))
                know[e][f] = v
                sn = snap.get((f, v))
                if sn is not None:
                    for g, gv in sn[0].items():
                        if know[e][g] < gv:
                            know[e][g] = gv
                    know_dma[e] |= sn[1]
            waits[op.id] = w
            if (not op.is_dma) and op.signal:
                snap[(e, op.semval)] = (dict(know[e]), set(know_dma[e]))
        return waits

    def run_block(self, final_wait_ops=()):
        nc = self.nc
        waits = self.plan()
        with ExitStack() as st:
            esem = {e: st.enter_context(nc.semaphore("s_" + e)) for e in self.ENGS}
            dsem = {}
            for (e, k) in sorted(self.dma_last.keys()):
                dsem[(e, k)] = st.enter_context(nc.semaphore("d_%s%d" % (e, k)))
            block = st.enter_context(nc.Block())

            def body(ename, h):
                for op in self.by_eng[ename]:
                    for (kind, a, v) in waits[op.id]:
                        h.wait_ge(dsem[a] if kind == "dma" else esem[a], v)
                    ins = op.fn(h)
                    if op.is_dma and op.sem[0] == "cc":
                        ins.then_inc(dsem[op.sem])
                    elif op.is_dma:
                        ins.then_inc(dsem[op.sem], 16)
                    elif op.signal:
                        ins.then_inc(esem[ename], 1)
                for fop in final_wait_ops:
                    if fop.eng == ename:
                        h.wait_ge(dsem[fop.sem], fop.semval)

            @block.tensor
            def _(h):
                body("pe", h)

            @block.scalar
            def _(h):
                body("act", h)

            @block.vector
            def _(h):
                body("dve", h)

            @block.gpsimd
            def _(h):
                body("pool", h)

            @block.sync
            def _(h):
                body("sp", h)


class Arena:
    def __init__(self, t, nwords, base=0):
        self.t = t
        self.n = base + nwords
        self.top = base

    def mark(self):
        return self.top

    def release(self, m):
        self.top = m

    def alloc(self, shape, dt):
        sz = mybir.dt.size(dt)
        n = 1
        for s in shape[1:]:
            n *= s
        words = (n * sz + 3) // 4
        words = (words + 7) // 8 * 8
        assert self.top + words <= self.n, ("SBUF arena overflow", self.top, words, self.n)
        ap = self.t[0:shape[0], self.top:self.top + words]
        self.top += words
        if dt != F32:
            ap = ap.bitcast(dt)
        ap = ap[:, 0:n]
        if len(shape) > 2:
            names = " ".join("d%d" % i for i in range(len(shape) - 1))
            kw = {"d%d" % i: shape[i + 1] for i in range(len(shape) - 1)}
            ap = ap.rearrange("p (%s) -> p %s" % (names, names), **kw)
        return ap


class KB:
    def __init__(self, nc, arena, psum):
        self.nc = nc
        self.S = Sched(nc)
        self.A = arena
        self.psum = psum

    def bank(self, b, n=512, dt=F32, off=0):
        ap = self.psum[:, b * 512:(b + 1) * 512]
        if dt != F32:
            ap = ap.bitcast(dt)
        return ap[:, off:off + n]

    def mm(self, out, lhsT, rhs, start=True, stop=True):
        rd = [lhsT, rhs] + ([] if start else [out])
        return self.S.add("pe", lambda e: e.matmul(out, lhsT=lhsT, rhs=rhs, start=start, stop=stop), rd, [out])

    def tr(self, out, in_, ident):
        return self.S.add("pe", lambda e: e.transpose(out=out, in_=in_, identity=ident), [in_, ident], [out])

    def act(self, out, in_, func, scale=None, bias=None, accum=None):
        kw = {}
        rd = [in_]
        wr = [out]
        if scale is not None:
            kw["scale"] = scale
            rd.append(scale)
        if bias is not None:
            kw["bias"] = bias
            rd.append(bias)
        if accum is not None:
            kw["accum_out"] = accum
            wr.append(accum)
        return self.S.add("act", lambda e: e.activation(out=out, in_=in_, func=func, **kw), rd, wr)

    def tt(self, eng, out, a, b, op):
        return self.S.add(eng, lambda e: e.tensor_tensor(out=out, in0=a, in1=b, op=op), [a, b], [out])

    def ts(self, eng, out, a, s1, op0, s2=None, op1=None):
        if op1 is None:
            return self.S.add(eng, lambda e: e.tensor_scalar(out=out, in0=a, scalar1=s1, scalar2=None, op0=op0), [a, s1], [out])
        return self.S.add(eng, lambda e: e.tensor_scalar(out=out, in0=a, scalar1=s1, scalar2=s2, op0=op0, op1=op1), [a, s1, s2], [out])

    def stt(self, out, a, scalar, b, op0, op1):
        return self.S.add("dve", lambda e: e.scalar_tensor_tensor(out=out, in0=a, scalar=scalar, in1=b, op0=op0, op1=op1), [a, scalar, b], [out])

    def cp(self, eng, out, in_):
        if eng == "act":
            return self.S.add("act", lambda e: e.copy(out=out, in_=in_), [in_], [out])
        return self.S.add(eng, lambda e: e.tensor_copy(out=out, in_=in_), [in_], [out])

    def memset(self, eng, ap, val):
        return self.S.add(eng, lambda e: e.memset(ap, val), [], [ap])

    def dma(self, q, out, in_, **kw):
        return self.S.add(q, lambda e: e.dma_start(out=out, in_=in_, **kw), [in_], [out], dma=True)

    def recip(self, out, in_):
        return self.S.add("dve", lambda e: e.reciprocal(out=out, in_=in_), [in_], [out])

    def iota(self, out, pattern, base, cm):
        return self.S.add("pool", lambda e: e.iota(out, pattern=pattern, base=base, channel_multiplier=cm), [], [out])

    def asel(self, out, in_, pattern, op, fill, base, cm):
        return self.S.add("pool", lambda e: e.affine_select(out=out, in_=in_, pattern=pattern, compare_op=op, fill=fill, base=base, channel_multiplier=cm), [in_], [out])


RG = [[0, 1], [2, 3], [4, 5], [6, 7]]
LN_G = [math.log1p(-2.0 ** (-5 - hh)) for hh in range(4)]
LN_DK = math.log(128.0 ** -0.5)


class Prog:
    def __init__(self, NCH=16, stages=("l0mix", "moe0", "l1mix", "moe1", "final"), dumps=(), n_cores=8):
        self.NCH = NCH
        self.T = NCH * 128
        self.NTB = self.T // 512
        self.stages = tuple(stages)
        self.dumps = set(dumps)
        self.dump_t = {}
        self.n_cores = n_cores
        self.rg = RG[: n_cores // 2]
        self.nc = bass.Bass("TRN2", target_bir_lowering=False)
        self.out_ops = []
        self.build()

    def din(self, name, shape):
        return self.nc.dram_tensor(name, list(shape), F32, kind="ExternalInput").ap()

    def dint(self, name, shape, dt):
        return self.nc.dram_tensor(name, list(shape), dt).ap()

    def dump(self, name, ap, dram_shape, pattern=None, **kw):
        if name not in self.dumps:
            return
        t = self.nc.dram_tensor("dbg_" + name, list(dram_shape), ap.dtype, kind="ExternalOutput").ap()
        self.dump_t[name] = (list(dram_shape), ap.dtype)
        dst = t if pattern is None else t.rearrange(pattern, **kw)
        self.out_ops.append(self.K.dma("sp", dst, ap))

    def build(self):
        nc = self.nc
        T = self.T
        i = self.inp = {}
        for name, shape in [("x", [T, D]), ("flag", [128, 1]), ("mix_norm", [2, D]), ("ffn_norm", [2, D]),
                            ("final_norm", [D]), ("ab_w_in", [1, D, 2560]), ("ab_w_out", [1, D, D]),
                            ("s5_a_re", [1, 32, 64]), ("s5_a_im", [1, 32, 64]), ("s5_b_re", [1, 32, 64, 16]),
                            ("s5_b_im", [1, 32, 64, 16]), ("s5_c_re", [1, 32, 16, 64]), ("s5_c_im", [1, 32, 16, 64]),
                            ("s5_d", [1, 32, 16]), ("s5_log_dt", [1, 32]), ("s5_w_glu", [1, 512, 512]),
                            ("s5_b_glu", [1, 512]), ("c_w_qkv", [1, D, 3072]), ("c_w_out", [1, D, D]),
                            ("moe_w_group", [2, D, 4]), ("moe_b_group", [2, 4]), ("moe_w_router", [2, D, 4, 4]),
                            ("moe_b_router", [2, 4, 4]), ("moe_w_gate", [2, 4, 4, D, 512]),
                            ("moe_w_up", [2, 4, 4, D, 512]), ("moe_w_down", [2, 4, 4, 512, D])]:
            i[name] = self.din(name, shape)
        self.y_out = nc.dram_tensor("y", [T, D], F32, kind="ExternalOutput").ap()
        ARENA_WORDS = 51200
        with ExitStack() as st:
            arena_t = st.enter_context(nc.sbuf_tensor("arena", [128, ARENA_WORDS], F32))
            psum_t = st.enter_context(nc.psum_tensor("psum", [128, 4096], F32))
            self.A = A = Arena(arena_t, ARENA_WORDS)
            self.K = K = KB(nc, A, psum_t)
            self.setup_consts()
            if "l0mix" in self.stages:
                self.l0mix()
            else:
                K.dma("sp", self.h, i["x"].rearrange("(c p) d -> p c d", p=128))
            self.dump("h0m", self.h, [T, D], "(c p) d -> p c d", p=128)
            if "moe0" in self.stages:
                self.moe(0)
            self.dump("h0", self.h, [T, D], "(c p) d -> p c d", p=128)
            if "l1mix" in self.stages:
                self.l1mix()
            self.dump("h1m", self.h, [T, D], "(c p) d -> p c d", p=128)
            if "moe1" in self.stages:
                self.moe(1)
            self.dump("h1", self.h, [T, D], "(c p) d -> p c d", p=128)
            self.final()
            K.S.run_block(final_wait_ops=self.out_ops)

    def setup_consts(self):
        K, A = self.K, self.A
        self.ident_f = A.alloc([128, 128], F32)
        self.ident_b = A.alloc([128, 128], BF16)
        self.ones_b = A.alloc([128, 128], BF16)
        self.flag = A.alloc([128, 1], F32)
        self.gB = A.alloc([128, D], F32)
        self.stats = A.alloc([128, 32], F32)
        self.pidx = A.alloc([128, 1], F32)
        self.signc = A.alloc([128, 1], F32)
        self.cst = A.alloc([128, 16], F32)
        K.memset("pool", self.ident_f, 1.0)
        K.asel(self.ident_f, self.ident_f, [[-1, 128]], ALU.is_equal, 0.0, 0, 1)
        K.cp("dve", self.ident_b, self.ident_f)
        K.memset("pool", self.ones_b, 1.0)
        self.pswap = A.alloc([128, 128], F32)
        K.cp("dve", self.pswap[:, 0:64], self.ident_f[:, 64:128])
        K.cp("dve", self.pswap[:, 64:128], self.ident_f[:, 0:64])
        K.dma("sp", self.flag, self.inp["flag"])
        pi = A.alloc([128, 1], I32)
        K.iota(pi, [[0, 1]], 0, 1)
        K.cp("dve", self.pidx, pi)
        K.memset("pool", self.signc[0:64, :], -1.0)
        K.memset("pool", self.signc[64:128, :], 1.0)
        for hh in range(4):
            K.memset("pool", self.cst[:, hh:hh + 1], LN_G[hh])
            K.memset("pool", self.cst[:, 4 + hh:5 + hh], 127.0 * LN_G[hh] + LN_DK)
        K.memset("pool", self.cst[:, 8:9], LN_DK)
        K.memset("pool", self.cst[:, 9:10], 0.0)
        K.memset("pool", self.cst[:, 10:11], math.pi / 2)
        self.stat_rr = 0
        h_base = A.mark()
        self.h = A.alloc([128, 16, D], F32)[:, 0:self.NCH, :]
        self.HA = Arena(A.t, 16 * D, base=h_base)
        self.xnT = A.alloc([128, KC, self.T], BF16)
        self.NST = 3
        self.stage = [A.alloc([128, 2048], F32) for _ in range(self.NST)]
        self.st_rr = 0
        self.base_mark = A.mark()

    def stat_slot(self):
        s = self.stat_rr % 8
        self.stat_rr += 1
        return self.stats[:, s * 4:(s + 1) * 4]

    def load_gamma(self, vec_ap):
        self.K.dma("sp", self.gB, vec_ap.partition_broadcast(128))

    def rms(self, src, out, junk):
        K = self.K
        ss = self.stat_slot()
        K.act(junk, src, AF.Square, accum=ss[:, 0:1])
        K.ts("dve", ss[:, 1:2], ss[:, 0:1], 1.0 / D, ALU.mult, 1e-6, ALU.add)
        K.act(ss[:, 2:3], ss[:, 1:2], AF.Sqrt)
        K.recip(ss[:, 3:4], ss[:, 2:3])
        K.stt(out, src, ss[:, 3:4], self.gB, ALU.mult, ALU.mult)

    def wload(self, dst, src, shape, cast_eng="pool"):
        a, b = shape
        assert a * b <= 2048
        sl = self.stage[self.st_rr % self.NST]
        self.st_rr += 1
        v = sl[:, 0:a * b].rearrange("p (a b) -> p a b", a=a)
        self.K.dma("sp", v, src)
        self.K.cp(cast_eng, dst, v)

    def to_xnT(self, xn_bf, c, bk):
        K = self.K
        pT = K.bank(bk, 1024, BF16)
        for k in range(KC):
            K.tr(pT[:, k * 128:(k + 1) * 128], xn_bf[:, k * 128:(k + 1) * 128], self.ident_b)
        K.cp("act", self.xnT[:, :, c * 128:(c + 1) * 128], pT.rearrange("p (k t) -> p k t", k=KC))

    def sin_rr(self, out, ang, tmp):
        K = self.K
        K.ts("dve", tmp, ang, 1.0 / TWO_PI, ALU.mult, MAGIC, ALU.add)
        K.ts("dve", tmp, tmp, MAGIC, ALU.subtract, -TWO_PI, ALU.mult)
        K.tt("dve", tmp, ang, tmp, ALU.add)
        K.ts("dve", tmp, tmp, -3.14159, ALU.max, 3.14159, ALU.min)
        K.act(out, tmp, AF.Sin)

    def final(self):
        K, A = self.K, self.A
        m = A.mark()
        if "final" in self.stages:
            self.load_gamma(self.inp["final_norm"])
        yo = [A.alloc([128, D], F32) for _ in range(2)]
        junk = A.alloc([128, D], BF16)
        for c in range(self.NCH):
            if "final" in self.stages:
                self.rms(self.h[:, c, :], yo[c % 2], junk)
                src = yo[c % 2]
            else:
                src = self.h[:, c, :]
            self.out_ops.append(K.dma("sp", self.y_out[c * 128:(c + 1) * 128, :], src))
        A.release(m)

    def l0mix(self):
        K, A, HA, NCH, T, NTB = self.K, self.A, self.HA, self.NCH, self.T, self.NTB
        nc = self.nc
        inp = self.inp
        xnT = self.xnT
        m0 = A.mark()
        hm0 = HA.mark()
        uT = A.alloc([128, 4, T], BF16)
        mA = A.mark()
        qT = A.alloc([128, 4, T], BF16)
        kT = A.alloc([128, 4, T], BF16)
        vtm = A.alloc([128, NCH, 512], BF16)
        sg_d = self.dint("sg_d", [NCH, 128, 512], BF16)
        sgb = [A.alloc([128, 512], BF16) for _ in range(2)]
        m1 = A.mark()
        hm1 = HA.mark()
        if "s5" in self.stages:
            self.s5_setup()

        xc = [HA.alloc([128, D], F32) for _ in range(2)]
        xnb = [HA.alloc([128, D], BF16) for _ in range(2)]
        junk = HA.alloc([128, D], BF16)
        self.load_gamma(inp["mix_norm"][0])
        for c in range(NCH):
            K.dma("sp", xc[c % 2], inp["x"][c * 128:(c + 1) * 128, :])
            self.rms(xc[c % 2], xnb[c % 2], junk)
            self.to_xnT(xnb[c % 2], c, c % 2)
        HA.release(hm1)

        cosT = HA.alloc([128, T], F32)
        sinST = HA.alloc([128, T], F32)
        hm2 = HA.mark()
        ang = HA.alloc([128, T], F32)
        tmp = HA.alloc([128, T], F32)
        pos_i = HA.alloc([128, T], I32)
        ji = HA.alloc([128, 1], I32)
        jf = HA.alloc([128, 1], F32)
        inv = HA.alloc([128, 1], F32)
        offc = HA.alloc([128, 1], F32)
        K.iota(pos_i, [[1, T]], 0, 0)
        K.cp("dve", tmp, pos_i)
        K.iota(ji[0:64, :], [[0, 1]], 0, 1)
        K.iota(ji[64:128, :], [[0, 1]], 0, 1)
        K.cp("dve", jf, ji)
        K.act(inv, jf, AF.Exp, scale=-math.log(10000.0) / 64.0)
        K.ts("dve", offc, self.flag, float(T), ALU.mult)
        K.ts("dve", ang, tmp, offc, ALU.add, inv, ALU.mult)
        self.sin_rr(sinST, ang, tmp)
        K.ts("dve", sinST, sinST, self.signc, ALU.mult)
        K.ts("dve", ang, ang, math.pi / 2, ALU.add)
        self.sin_rr(cosT, ang, tmp)
        HA.release(hm2)

        Win = inp["ab_w_in"][0].rearrange("(k p) n -> p k n", p=128)
        wn = HA.alloc([128, KC, 512], BF16)
        ws = HA.alloc([128, KC, 512], BF16)
        t1 = [HA.alloc([128, 512], F32) for _ in range(2)]
        t2 = [HA.alloc([128, 512], F32) for _ in range(2)]
        wvg = HA.alloc([128, KC, 1024], BF16)
        it = 0
        for base, dst in ((0, qT), (512, kT)):
            for j in range(2):
                self.wload(wn[:, 4 * j:4 * j + 4, :], Win[:, 4 * j:4 * j + 4, base:base + 512], (4, 512))
            wn5 = wn.rearrange("p k (h two q) -> p k h two q", h=4, two=2)
            ws5 = ws.rearrange("p k (h two q) -> p k h two q", h=4, two=2)
            K.cp("pool", ws5[:, :, :, 0, :], wn5[:, :, :, 1, :])
            K.cp("act", ws5[:, :, :, 1, :], wn5[:, :, :, 0, :])
            for hd in range(4):
                hsl = slice(hd * 128, (hd + 1) * 128)
                for tb in range(NTB):
                    pa = K.bank(2 + it % 2)
                    pb = K.bank(4 + it % 2)
                    tsl = slice(tb * 512, (tb + 1) * 512)
                    for kc in range(KC):
                        K.mm(pa, wn[:, kc, hsl], xnT[:, kc, tsl], kc == 0, kc == KC - 1)
                    for kc in range(KC):
                        K.mm(pb, ws[:, kc, hsl], xnT[:, kc, tsl], kc == 0, kc == KC - 1)
                    K.tt("dve", t1[it % 2], pa, cosT[:, tsl], ALU.mult)
                    K.tt("dve", t2[it % 2], pb, sinST[:, tsl], ALU.mult)
                    K.tt("pool", dst[:, hd, tsl], t1[it % 2], t2[it % 2], ALU.add)
                    it += 1
        for j in range(2):
            self.wload(wn[:, 4 * j:4 * j + 4, :], Win[:, 4 * j:4 * j + 4, 2048:2560], (4, 512))
        for cb in range(4):
            for tb in range(NTB):
                pa = K.bank(2 + it % 2)
                tsl = slice(tb * 512, (tb + 1) * 512)
                for kc in range(KC):
                    K.mm(pa, wn[:, kc, cb * 128:(cb + 1) * 128], xnT[:, kc, tsl], kc == 0, kc == KC - 1)
                K.cp("act", uT[:, cb, tsl], pa)
                it += 1
        for cg in range(2):
            for j in range(2):
                self.wload(wvg[:, 4 * j:4 * j + 4, cg * 512:(cg + 1) * 512],
                           Win[:, 4 * j:4 * j + 4, 1024 + cg * 512:1024 + (cg + 1) * 512], (4, 512))
        for c in range(NCH):
            pv = K.bank(2 + c % 2)
            pg = K.bank(4 + c % 2)
            csl = slice(c * 128, (c + 1) * 128)
            for kc in range(KC):
                K.mm(pv, xnT[:, kc, csl], wvg[:, kc, 0:512], kc == 0, kc == KC - 1)
            for kc in range(KC):
                K.mm(pg, xnT[:, kc, csl], wvg[:, kc, 512:1024], kc == 0, kc == KC - 1)
            K.cp("act", vtm[:, c, :], pv)
            K.act(sgb[c % 2], pg, AF.Silu)
            K.dma("sp", sg_d[c], sgb[c % 2])
        HA.release(hm1)
        self.dump("qT", qT, [4, 128, T], "h p t -> p h t")
        self.dump("kT", kT, [4, 128, T], "h p t -> p h t")
        self.dump("uT", uT, [4, 128, T], "h p t -> p h t")

        decT = HA.alloc([128, 4, 128], F32)
        xi = HA.alloc([128, 4], F32)
        zeta = HA.alloc([128, 4], F32)
        Sst = HA.alloc([128, 4, 128], F32)
        Sin = HA.alloc([128, 4, 128], F32)
        Sb = HA.alloc([128, 4, 128], BF16)
        kz = [HA.alloc([128, 4, 128], BF16) for _ in range(2)]
        dfi = HA.alloc([128, 128], I32)
        dff = HA.alloc([128, 128], F32)
        K.iota(dfi, [[1, 128]], 0, -1)
        K.cp("dve", dff, dfi)
        for hd in range(4):
            K.act(decT[:, hd, :], dff, AF.Exp, scale=LN_G[hd], bias=self.cst[:, 8:9])
            K.asel(decT[:, hd, :], decT[:, hd, :], [[1, 128]], ALU.is_ge, 0.0, 0, -1)
            K.act(xi[:, hd:hd + 1], self.pidx, AF.Exp, scale=LN_G[hd], bias=self.cst[:, hd:hd + 1])
            K.act(zeta[:, hd:hd + 1], self.pidx, AF.Exp, scale=-LN_G[hd], bias=self.cst[:, 4 + hd:5 + hd])
        K.memset("pool", Sst, 0.0)
        g128 = [math.exp(128.0 * LN_G[hd]) for hd in range(4)]

        def state_update(n):
            kzb = kz[n % 2]
            pz = K.bank(0, 512, BF16)
            csl = slice(n * 128, (n + 1) * 128)
            for hd in range(4):
                K.tr(pz[:, hd * 128:(hd + 1) * 128], kT[:, hd, csl], self.ident_b)
            for hd in range(4):
                K.act(kzb[:, hd, :], pz[:, hd * 128:(hd + 1) * 128], AF.Copy, scale=zeta[:, hd:hd + 1])
            pkv = K.bank(1)
            for hd in range(4):
                K.mm(pkv[:, hd * 128:(hd + 1) * 128], kzb[:, hd, :], vtm[:, n, hd * 128:(hd + 1) * 128])
            for hd in range(4):
                K.stt(Sst[:, hd, :], Sst[:, hd, :], g128[hd], pkv[:, hd * 128:(hd + 1) * 128], ALU.mult, ALU.add)

        do_s5p1 = "s5" in self.stages and os.environ.get("KSTOP") != "setup"
        if do_s5p1:
            s5_step, s5_fin = self.s5_pass1_begin(uT)
        for n in range(NCH):
            state_update(n)
            if do_s5p1:
                s5_step(n)
        NW = 512 + 32
        st_loc = self.dint("st_loc", [128, NW], F32)
        st_all = self.dint("st_all", [256, NW], F32)
        K.dma("sp", st_loc[:, 0:512], Sst.rearrange("p h e -> p (h e)"))
        if do_s5p1:
            s5_fin(st_loc)
        else:
            zz = HA.alloc([128, 32], F32)
            K.memset("pool", zz, 0.0)
            K.dma("sp", st_loc[:, 512:544], zz)
        K.S.add("pool", lambda e: e.collective_compute("AllGather", ALU.bypass, replica_groups=self.rg,
                                                       ins=[st_loc.opt()], outs=[st_all.opt()]),
                [st_loc], [st_all], dma="cc")
        K.dma("sp", Sin.rearrange("p h e -> p (h e)"), st_all[0:128, 0:512])
        K.ts("dve", Sst.rearrange("p h e -> p (h e)"), Sin.rearrange("p h e -> p (h e)"), self.flag, ALU.mult)

        PT = HA.alloc([128, 4, 128], BF16)
        tmpo = HA.alloc([128, 512], F32)
        o = HA.alloc([128, 512], F32)
        on = HA.alloc([128, 512], F32)
        ret = HA.alloc([128, 512], BF16)
        bst = HA.alloc([128, 24], F32)
        mv = HA.alloc([128, 8], F32)
        rs = HA.alloc([128, 4], F32)
        nb = HA.alloc([128, 4], F32)
        mv3 = mv.rearrange("p (h two) -> p h two", two=2)
        mergedT = xnT
        for n in range(NCH):
            csl = slice(n * 128, (n + 1) * 128)
            K.cp("act", Sb, Sst)
            ps_s = K.bank(2)
            for hd in range(4):
                K.mm(ps_s[:, hd * 128:(hd + 1) * 128], kT[:, hd, csl], qT[:, hd, csl])
            K.tt("dve", PT.rearrange("p h c -> p (h c)"), ps_s, decT.rearrange("p h c -> p (h c)"), ALU.mult)
            pO1 = K.bank(3)
            pO2 = K.bank(4)
            for hd in range(4):
                K.mm(pO1[:, hd * 128:(hd + 1) * 128], PT[:, hd, :], vtm[:, n, hd * 128:(hd + 1) * 128])
            for hd in range(4):
                K.mm(pO2[:, hd * 128:(hd + 1) * 128], qT[:, hd, csl], Sb[:, hd, :])
            for hd in range(4):
                K.act(tmpo[:, hd * 128:(hd + 1) * 128], pO2[:, hd * 128:(hd + 1) * 128], AF.Copy, scale=xi[:, hd:hd + 1])
            K.tt("dve", o, tmpo, pO1, ALU.add)
            for hd in range(4):
                oh = o[:, hd * 128:(hd + 1) * 128]
                bs = bst[:, hd * 6:(hd + 1) * 6]
                K.S.add("dve", lambda e, oh=oh, bs=bs: e.bn_stats(out=bs, in_=oh), [oh], [bs])
                mvh = mv[:, hd * 2:(hd + 1) * 2]
                K.S.add("dve", lambda e, mvh=mvh, bs=bs: e.bn_aggr(out=mvh, in_=bs), [bs], [mvh])
            K.ts("dve", rs, mv3[:, :, 1], 1e-6, ALU.add)
            K.act(rs, rs, AF.Sqrt)
            K.recip(rs, rs)
            K.stt(nb, mv3[:, :, 0], -1.0, rs, ALU.mult, ALU.mult)
            for hd in range(4):
                K.ts("dve", on[:, hd * 128:(hd + 1) * 128], o[:, hd * 128:(hd + 1) * 128], rs[:, hd:hd + 1], ALU.mult,
                     nb[:, hd:hd + 1], ALU.add)
            K.dma("sp", sgb[n % 2], sg_d[n])
            K.tt("pool", ret, on, sgb[n % 2], ALU.mult)
            pR = K.bank(5, 512, BF16)
            for hd in range(4):
                K.tr(pR[:, hd * 128:(hd + 1) * 128], ret[:, hd * 128:(hd + 1) * 128], self.ident_b)
            K.cp("act", mergedT[:, 0:4, csl], pR.rearrange("p (h t) -> p h t", h=4))
            if n + 1 < NCH:
                state_update(n)
        self.dump("retT", mergedT[:, 0:4, :], [4, 128, T], "h p t -> p h t")
        HA.release(hm1)
        A.release(mA)
        if "s5" in self.stages and os.environ.get("KSTOP") not in ("setup", "pass1"):
            self.s5_pass2(uT, st_all, mergedT)
        else:
            zt = HA.alloc([128, 4, 128], BF16)
            K.memset("pool", zt, 0.0)
            for n in range(NCH):
                K.cp("pool", mergedT[:, 4:8, n * 128:(n + 1) * 128], zt)
        self.dump("mergedT", mergedT, [8, 128, T], "h p t -> p h t")
        A.release(m0)
        HA.release(hm0)

        m2 = A.mark()
        wo = A.alloc([128, KC, D], BF16)
        Wout = inp["ab_w_out"][0].rearrange("(k p) n -> p k n", p=128)
        for cg in range(2):
            for j in range(2):
                self.wload(wo[:, 4 * j:4 * j + 4, cg * 512:(cg + 1) * 512],
                           Wout[:, 4 * j:4 * j + 4, cg * 512:(cg + 1) * 512], (4, 512))
        for c in range(NCH):
            csl = slice(c * 128, (c + 1) * 128)
            K.dma("sp", self.h[:, c, :], inp["x"][c * 128:(c + 1) * 128, :])
            for dh in range(2):
                po = K.bank(6 + dh)
                dsl = slice(dh * 512, (dh + 1) * 512)
                for fb in range(8):
                    K.mm(po, mergedT[:, fb, csl], wo[:, fb, dsl], fb == 0, fb == 7)
                K.tt("dve", self.h[:, c, dsl], po, self.h[:, c, dsl], ALU.add)
        A.release(m2)

    def cmul(self, Y, Ac, Bc, X, tA, tB):
        K = self.K
        psw = K.bank(4)[:, 0:32]
        K.mm(psw, self.pswap, X)
        K.tt("dve", tA, X, Ac, ALU.mult)
        K.tt("dve", tB, psw, Bc, ALU.mult)
        K.tt("dve", Y, tA, tB, ALU.add)

    def s5_setup(self):
        K, HA = self.K, self.HA
        inp = self.inp
        d = self.s5d = {}
        for nm, n, dt in [("TA", 2048, BF16), ("TB", 2048, BF16), ("TBn", 2048, BF16), ("BB", 4096, BF16),
                          ("A2", 4096, BF16), ("B2", 4096, BF16), ("C1", 4096, BF16), ("C2", 4096, BF16),
                          ("L", 192, F32)]:
            d[nm] = self.dint("s5_" + nm, [128, n], dt)
        hm = HA.mark()

        def tl():
            return HA.alloc([128, 32], F32)

        X = HA.alloc([32, 256], F32)
        a_re = inp["s5_a_re"][0]
        a_im = inp["s5_a_im"][0]
        K.dma("sp", X[:, 0:64], a_re)
        K.dma("sp", X[:, 64:128], a_re)
        K.dma("sp", X[:, 128:192], a_im)
        K.dma("sp", X[:, 192:256], a_im)
        pb = K.bank(6)
        K.tr(pb[:, 0:32], X[:, 0:128], self.ident_f[0:32, 0:32])
        K.tr(pb[:, 32:64], X[:, 128:256], self.ident_f[0:32, 0:32])
        are2, aim2, ldt, dt, zr, zi = tl(), tl(), tl(), tl(), tl(), tl()
        K.cp("act", are2, pb[:, 0:32])
        K.cp("act", aim2, pb[:, 32:64])
        K.dma("sp", ldt, inp["s5_log_dt"][0].partition_broadcast(128))
        K.act(dt, ldt, AF.Exp)
        K.tt("dve", zr, are2, dt, ALU.mult)
        K.tt("dve", zi, aim2, dt, ALU.mult)
        self.s5_zr, self.s5_zi = zr, zi
        Lc = HA.alloc([128, 6, 32], F32)
        tmp, angk, mg, sn, cs = tl(), tl(), tl(), tl(), tl()

        def lam_pow(kk, Ac, Bc):
            K.ts("dve", angk, zi, float(kk), ALU.mult)
            self.sin_rr(sn, angk, tmp)
            K.ts("dve", angk, angk, math.pi / 2, ALU.add)
            self.sin_rr(cs, angk, tmp)
            K.act(mg, zr, AF.Exp, scale=float(kk))
            K.tt("dve", Ac, mg, cs, ALU.mult)
            K.stt(Bc, mg, self.signc, sn, ALU.mult, ALU.mult)

        for j, kk in enumerate((1, 127, 128)):
            lam_pow(kk, Lc[:, 2 * j, :], Lc[:, 2 * j + 1, :])
        K.dma("sp", d["L"], Lc.rearrange("p a g -> p (a g)"))
        lre, lim_s = Lc[:, 0, :], Lc[:, 1, :]
        nr, ni, den, cre, cis, t3 = tl(), tl(), tl(), tl(), tl(), tl()
        K.ts("dve", nr, lre, -1.0, ALU.add)
        K.ts("dve", ni, lim_s, self.signc, ALU.mult)
        K.tt("dve", den, are2, are2, ALU.mult)
        K.tt("dve", t3, aim2, aim2, ALU.mult)
        K.tt("dve", den, den, t3, ALU.add)
        K.recip(den, den)
        K.tt("dve", cre, nr, are2, ALU.mult)
        K.tt("dve", t3, ni, aim2, ALU.mult)
        K.tt("dve", cre, cre, t3, ALU.add)
        K.tt("dve", cre, cre, den, ALU.mult)
        K.tt("dve", cis, ni, are2, ALU.mult)
        K.tt("dve", t3, nr, aim2, ALU.mult)
        K.tt("dve", cis, cis, t3, ALU.subtract)
        K.tt("dve", cis, cis, den, ALU.mult)
        K.ts("dve", cis, cis, self.signc, ALU.mult)
        hm_small = HA.mark()
        M1 = HA.alloc([128, 32, 16], F32)
        M2 = HA.alloc([128, 32, 16], F32)
        bre = inp["s5_b_re"][0].rearrange("g p h -> p g h")
        bim = inp["s5_b_im"][0].rearrange("g p h -> p g h")
        K.dma("sp", M1[0:64], bre)
        K.dma("sp", M1[64:128], bim)
        K.dma("sp", M2[0:64], bim)
        K.dma("sp", M2[64:128], bre)
        bbX = HA.alloc([128, 32, 16], F32)
        tmb = HA.alloc([128, 32, 16], F32)
        K.tt("dve", bbX, M1, cre.unsqueeze(2).to_broadcast([128, 32, 16]), ALU.mult)
        K.tt("dve", tmb, M2, cis.unsqueeze(2).to_broadcast([128, 32, 16]), ALU.mult)
        K.tt("dve", bbX, bbX, tmb, ALU.add)
        pb2 = K.bank(7)
        for cb in range(4):
            K.tr(pb2[:, cb * 128:(cb + 1) * 128], bbX[:, cb * 8:(cb + 1) * 8, :].rearrange("p g h -> p (g h)"), self.ident_f)
        bbT = HA.alloc([128, 4, 128], F32)
        K.cp("act", bbT.rearrange("p c q -> p (c q)"), pb2)
        maskc = HA.alloc([128, 8], F32)
        K.memset("pool", maskc, 1.0)
        K.asel(maskc, maskc, [[-16, 8]], ALU.is_ge, 0.0, 0, 1)
        K.asel(maskc, maskc, [[16, 8]], ALU.is_ge, 0.0, 15, -1)
        BB = HA.alloc([128, 4, 8, 128], BF16)
        K.tt("dve", BB, bbT.unsqueeze(2).to_broadcast([128, 4, 8, 128]),
             maskc.unsqueeze(1).unsqueeze(3).to_broadcast([128, 4, 8, 128]), ALU.mult)
        K.dma("sp", d["BB"], BB.rearrange("p a b c -> p (a b c)"))
        cre_t = HA.alloc([128, 4, 64], F32)
        cim_t = HA.alloc([128, 4, 64], F32)
        K.dma("sp", cre_t, inp["s5_c_re"][0].rearrange("(cb gl) h p -> (gl h) cb p", cb=4))
        K.dma("sp", cim_t, inp["s5_c_im"][0].rearrange("(cb gl) h p -> (gl h) cb p", cb=4))
        CC1 = HA.alloc([128, 4, 128], F32)
        CC2 = HA.alloc([128, 4, 128], F32)
        K.cp("dve", CC1[:, :, 0:64], cre_t)
        K.ts("dve", CC1[:, :, 64:128], cim_t, -1.0, ALU.mult)
        K.ts("dve", CC2[:, :, 0:64], cim_t, -1.0, ALU.mult)
        K.ts("dve", CC2[:, :, 64:128], cre_t, -1.0, ALU.mult)
        mask3 = HA.alloc([128, 8, 128], F32)
        K.memset("pool", mask3, 0.0)
        for a in range(8):
            K.memset("pool", mask3[:, a, a * 16:(a + 1) * 16], 1.0)
        CmX = HA.alloc([128, 4, 128], F32)
        Cp = HA.alloc([128, 4, 8, 128], BF16)
        for CC, nm, bk in ((CC1, "C1", 6), (CC2, "C2", 7)):
            pbc = K.bank(bk)
            for cb in range(4):
                K.tr(pbc[:, cb * 128:(cb + 1) * 128], CC[:, cb, :], self.ident_f)
            K.cp("act", CmX.rearrange("p c q -> p (c q)"), pbc)
            K.tt("dve", Cp, CmX.unsqueeze(2).to_broadcast([128, 4, 8, 128]),
                 mask3.unsqueeze(1).to_broadcast([128, 4, 8, 128]), ALU.mult)
            K.dma("sp", d[nm], Cp.rearrange("p a b c -> p (a b c)"))
        HA.release(hm_small)
        tgi = HA.alloc([128, 128], I32)
        tg = HA.alloc([128, 128], F32)
        K.iota(tgi, [[1, 128]], 0, 0)
        K.cp("dve", tg, tgi)
        ang2 = HA.alloc([128, 8, 128], F32)
        tmp2 = HA.alloc([128, 8, 128], F32)
        sn2 = HA.alloc([128, 8, 128], F32)
        cs2 = HA.alloc([128, 8, 128], F32)
        mg2 = HA.alloc([128, 8, 128], F32)
        A2p = HA.alloc([128, 8, 128], BF16)
        B2p = HA.alloc([128, 8, 128], BF16)
        tgb = tg.unsqueeze(1).to_broadcast([128, 8, 128])
        for cb in range(4):
            gsl = slice(cb * 8, (cb + 1) * 8)
            K.tt("dve", ang2, zi[:, gsl].unsqueeze(2).to_broadcast([128, 8, 128]), tgb, ALU.mult)
            self.sin_rr(sn2, ang2, tmp2)
            K.ts("dve", ang2, ang2, math.pi / 2, ALU.add)
            self.sin_rr(cs2, ang2, tmp2)
            K.tt("dve", mg2, zr[:, gsl].unsqueeze(2).to_broadcast([128, 8, 128]), tgb, ALU.mult)
            K.act(mg2, mg2, AF.Exp)
            K.tt("dve", A2p, mg2, cs2, ALU.mult)
            K.tt("dve", B2p, mg2, sn2, ALU.mult)
            K.dma("sp", d["A2"][:, cb * 1024:(cb + 1) * 1024], A2p.rearrange("p g t -> p (g t)"))
            K.dma("sp", d["B2"][:, cb * 1024:(cb + 1) * 1024], B2p.rearrange("p g t -> p (g t)"))
        HA.release(hm_small)
        zrr = HA.alloc([128, 32, 64], F32)
        zir = HA.alloc([128, 32, 64], F32)
        angT = HA.alloc([128, 2048], F32)
        tmpT = HA.alloc([128, 2048], F32)
        snT = HA.alloc([128, 2048], F32)
        ntc = HA.alloc([128, 1], F32)
        Tb = [HA.alloc([128, 2048], BF16) for _ in range(3)]
        K.dma("sp", zrr.rearrange("p g q -> p (g q)"), a_re.rearrange("g p -> (g p)").partition_broadcast(128))
        K.dma("sp", zir.rearrange("p g q -> p (g q)"), a_im.rearrange("g p -> (g p)").partition_broadcast(128))
        dtb = dt.unsqueeze(2).to_broadcast([128, 32, 64])
        K.tt("dve", zrr, zrr, dtb, ALU.mult)
        K.tt("dve", zir, zir, dtb, ALU.mult)
        zrf = zrr.rearrange("p g q -> p (g q)")
        zif = zir.rearrange("p g q -> p (g q)")
        K.ts("dve", angT, zif, self.pidx, ALU.mult)
        self.sin_rr(snT, angT, tmpT)
        K.ts("dve", angT, angT, math.pi / 2, ALU.add)
        self.sin_rr(zif, angT, tmpT)
        K.ts("dve", ntc, self.pidx, -1.0, ALU.mult)
        K.act(tmpT, zrf, AF.Exp, scale=ntc)
        K.tt("dve", Tb[0], tmpT, zif, ALU.mult)
        K.tt("dve", Tb[2], tmpT, snT, ALU.mult)
        K.ts("dve", Tb[1], Tb[2], -1.0, ALU.mult)
        K.dma("sp", d["TA"], Tb[0])
        K.dma("sp", d["TB"], Tb[1])
        K.dma("sp", d["TBn"], Tb[2])
        HA.release(hm)

    def s5_pass1_begin(self, uT):
        K, HA, NCH = self.K, self.HA, self.NCH
        d = self.s5d
        hm = HA.mark()
        TA = HA.alloc([128, 32, 64], BF16)
        TB = HA.alloc([128, 32, 64], BF16)
        TBn = HA.alloc([128, 32, 64], BF16)
        BB = HA.alloc([128, 4, 8, 128], BF16)
        Lc = HA.alloc([128, 6, 32], F32)
        K.dma("sp", TA.rearrange("p g q -> p (g q)"), d["TA"])
        K.dma("sp", TB.rearrange("p g q -> p (g q)"), d["TB"])
        K.dma("sp", TBn.rearrange("p g q -> p (g q)"), d["TBn"])
        K.dma("sp", BB.rearrange("p a b c -> p (a b c)"), d["BB"])
        K.dma("sp", Lc.rearrange("p a g -> p (a g)"), d["L"])
        PA = [HA.alloc([128, 4, 8, 2, 64], BF16) for _ in range(2)]
        PB = [HA.alloc([128, 4, 8, 2, 64], BF16) for _ in range(2)]
        self.pa_d = self.dint("s5_pa", [NCH, 128, 4096], BF16)
        self.pb_d = self.dint("s5_pb", [NCH, 128, 4096], BF16)
        X = HA.alloc([128, 32], F32)
        Zs = HA.alloc([128, 32], F32)
        t1, t2, tA, tB = (HA.alloc([128, 32], F32) for _ in range(4))
        K.memset("pool", X, 0.0)
        flat5 = "p a b c d -> p (a b c d)"

        def step(n):
            csl = slice(n * 128, (n + 1) * 128)
            pa, pb = PA[n % 2], PB[n % 2]
            for cb in range(4):
                pbu = self.K.psum[:, 6 * 512:8 * 512]
                K.mm(pbu[:, 0:512], uT[:, cb, csl], BB[:, cb, 0:4, :].rearrange("p g q -> p (g q)"))
                K.mm(pbu[:, 512:1024], uT[:, cb, csl], BB[:, cb, 4:8, :].rearrange("p g q -> p (g q)"))
                bu4 = pbu.rearrange("p (g r q) -> p g r q", g=8, r=2)
                gs = slice(cb * 8, (cb + 1) * 8)
                K.tt("dve", pa[:, cb], bu4, TA[:, gs, :].unsqueeze(2).to_broadcast([128, 8, 2, 64]), ALU.mult)
                K.tt("dve", pb[:, cb, :, 0, :], bu4[:, :, 1, :], TBn[:, gs, :], ALU.mult)
                K.tt("dve", pb[:, cb, :, 1, :], bu4[:, :, 0, :], TB[:, gs, :], ALU.mult)
            K.dma("sp", self.pa_d[n], pa.rearrange(flat5))
            K.dma("sp", self.pb_d[n], pb.rearrange(flat5))
            pz = K.bank(5)[:, 0:32]
            for g in range(32):
                cb, gl = divmod(g, 8)
                K.mm(pz[:, g:g + 1], pa[:, cb, gl].rearrange("p r q -> p (r q)"), self.ones_b[:, 0:1], True, False)
                K.mm(pz[:, g:g + 1], pb[:, cb, gl].rearrange("p r q -> p (r q)"), self.ones_b[:, 0:1], False, True)
            K.cp("act", Zs, pz)
            self.cmul(t1, Lc[:, 2, :], Lc[:, 3, :], Zs, tA, tB)
            self.cmul(t2, Lc[:, 4, :], Lc[:, 5, :], X, tA, tB)
            K.tt("dve", X, t1, t2, ALU.add)

        def finish(st_loc):
            K.dma("sp", st_loc[:, 512:544], X)
            HA.release(hm)

        return step, finish

    def s5_pass2(self, uT, st_all, mergedT):
        K, A, HA, NCH = self.K, self.A, self.HA, self.NCH
        d = self.s5d
        inp = self.inp
        hm = HA.mark()
        A2 = HA.alloc([128, 32, 128], BF16)
        B2 = HA.alloc([128, 32, 128], BF16)
        C1 = HA.alloc([128, 32, 128], BF16)
        C2 = HA.alloc([128, 32, 128], BF16)
        for t_, nm in ((A2, "A2"), (B2, "B2"), (C1, "C1"), (C2, "C2")):
            K.dma("sp", t_.rearrange("p g t -> p (g t)"), d[nm])
        XA = HA.alloc([128, 32, 128], BF16)
        XB = HA.alloc([128, 32, 128], BF16)
        Eall = HA.alloc([128, 32, 128], BF16)
        K.memset("pool", Eall, 1.0)
        K.asel(Eall, Eall, [[-1, 32], [0, 128]], ALU.is_equal, 0.0, 0, 1)
        PA = [A.alloc([128, 4, 8, 2, 64], BF16) for _ in range(2)]
        PB = [A.alloc([128, 4, 8, 2, 64], BF16) for _ in range(2)]
        Lc = A.alloc([128, 6, 32], F32)
        K.dma("sp", Lc.rearrange("p a g -> p (a g)"), d["L"])
        Tri = A.alloc([128, 128], BF16)
        K.memset("pool", Tri, 1.0)
        K.asel(Tri, Tri, [[1, 128]], ALU.is_ge, 0.0, 0, -1)
        wglu = A.alloc([128, 4, 512], BF16)
        self.wload(wglu, inp["s5_w_glu"][0].rearrange("(k p) n -> p k n", p=128), (4, 512))
        dcol = A.alloc([128, 4], F32)
        bglu = A.alloc([128, 4], F32)
        K.dma("sp", dcol, inp["s5_d"][0].rearrange("(cb gl) h -> (gl h) cb", cb=4), allow_slow_non_contiguous=True)
        K.dma("sp", bglu, inp["s5_b_glu"][0].rearrange("(cb p) -> p cb", cb=4), allow_slow_non_contiguous=True)
        X = A.alloc([128, 32], F32)
        Xin = A.alloc([128, 32], F32)
        Xc = A.alloc([128, 32], F32)
        Zc = A.alloc([128, 32], F32)
        tA = A.alloc([128, 32], F32)
        tB = A.alloc([128, 32], F32)
        XinT = A.alloc([128, 128], BF16)
        K.memset("pool", XinT, 0.0)
        yv = A.alloc([128, 4, 128], F32)
        y2 = A.alloc([128, 512], F32)
        sgm = A.alloc([128, 512], F32)
        gT = A.alloc([128, 4, 128], BF16)
        sig2 = A.alloc([128, 4, 128], F32)
        K.dma("sp", Xin, st_all[0:128, 512:544])
        K.ts("dve", X, Xin, self.flag, ALU.mult)
        flat5 = "p a b c d -> p (a b c d)"
        yvf = yv.rearrange("p c t -> p (c t)")
        for n in range(NCH):
            csl = slice(n * 128, (n + 1) * 128)
            pa, pb = PA[n % 2], PB[n % 2]
            K.dma("sp", pa.rearrange(flat5), self.pa_d[n])
            K.dma("sp", pb.rearrange(flat5), self.pb_d[n])
            self.cmul(Xc, Lc[:, 0, :], Lc[:, 1, :], X, tA, tB)
            pc = K.bank(3)
            K.tr(pc[0:32, 0:128], Xc, self.ident_f)
            K.cp("act", XinT[0:32], pc[0:32, 0:128])
            for gb in range(8):
                pz = K.bank(6 + gb % 2)
                for gi in range(4):
                    g = gb * 4 + gi
                    cb, gl = divmod(g, 8)
                    o_ = pz[:, gi * 128:(gi + 1) * 128]
                    K.mm(o_, pa[:, cb, gl].rearrange("p r q -> p (r q)"), Tri, True, False)
                    K.mm(o_, pb[:, cb, gl].rearrange("p r q -> p (r q)"), Tri, False, False)
                    K.mm(o_, XinT, Eall[:, g, :], False, True)
                gsl = slice(gb * 4, (gb + 1) * 4)
                K.tt("dve", XA[:, gsl, :].rearrange("p g t -> p (g t)"), pz, A2[:, gsl, :].rearrange("p g t -> p (g t)"), ALU.mult)
                K.tt("dve", XB[:, gsl, :].rearrange("p g t -> p (g t)"), pz, B2[:, gsl, :].rearrange("p g t -> p (g t)"), ALU.mult)
                K.cp("act", Zc[:, gsl], pz.rearrange("p (g t) -> p g t", g=4)[:, :, 127])
            self.cmul(X, Lc[:, 2, :], Lc[:, 3, :], Zc, tA, tB)
            py = K.bank(5)
            for cb in range(4):
                for gl in range(8):
                    g = cb * 8 + gl
                    K.mm(py[:, cb * 128:(cb + 1) * 128], C1[:, g, :], XA[:, g, :], gl == 0, False)
                    K.mm(py[:, cb * 128:(cb + 1) * 128], C2[:, g, :], XB[:, g, :], False, gl == 7)
            for cb in range(4):
                K.stt(yv[:, cb, :], uT[:, cb, csl], dcol[:, cb:cb + 1], py[:, cb * 128:(cb + 1) * 128], ALU.mult, ALU.add)
            K.tt("pool", y2, yvf, yvf, ALU.mult)
            K.ts("pool", y2, y2, 0.044715, ALU.mult, 1.0, ALU.add)
            K.tt("pool", y2, y2, yvf, ALU.mult)
            K.act(sgm, y2, AF.Sigmoid, scale=1.5957691216057308)
            K.tt("dve", gT.rearrange("p c t -> p (c t)"), yvf, sgm, ALU.mult)
            pzg = K.bank(4)
            for co in range(4):
                for ci in range(4):
                    K.mm(pzg[:, co * 128:(co + 1) * 128], wglu[:, ci, co * 128:(co + 1) * 128], gT[:, ci, :], ci == 0, ci == 3)
            for co in range(4):
                K.act(sig2[:, co, :], pzg[:, co * 128:(co + 1) * 128], AF.Sigmoid, bias=bglu[:, co:co + 1])
            K.tt("dve", mergedT[:, 4:8, csl], gT, sig2, ALU.mult)
        HA.release(hm)

    def moe(self, l):
        K, A, NCH, T, NTB = self.K, self.A, self.NCH, self.T, self.NTB
        inp = self.inp
        h, xnT = self.h, self.xnT
        m0 = A.mark()
        wg = [A.alloc([128, KC, 512], BF16) for _ in range(2)]
        wu = [A.alloc([128, KC, 512], BF16) for _ in range(2)]
        wd = [A.alloc([128, 4, D], BF16) for _ in range(2)]
        cw = A.alloc([128, NCH, 16], F32)
        wr32 = A.alloc([128, KC, 20], F32)
        b20 = A.alloc([128, 20], F32)
        xn32 = A.alloc([128, D], F32)
        xnb = A.alloc([128, D], BF16)
        xT32 = xn32.rearrange("p (k t) -> p k t", k=KC)
        rts = [A.alloc([128, 4, 64], F32) for _ in range(2)]
        hT = [A.alloc([128, 4, 512], BF16) for _ in range(2)]
        sgt = [A.alloc([128, 512], F32) for _ in range(2)]
        self.load_gamma(inp["ffn_norm"][l])
        K.dma("sp", wr32[:, :, 0:4], inp["moe_w_group"][l].rearrange("(k p) g -> p k g", p=128))
        K.dma("sp", wr32[:, :, 4:20], inp["moe_w_router"][l].rearrange("(k p) g e -> p k (g e)", p=128))
        K.dma("sp", b20[:, 0:4], inp["moe_b_group"][l].partition_broadcast(128))
        K.dma("sp", b20[:, 4:20], inp["moe_b_router"][l].rearrange("g e -> (g e)").partition_broadcast(128))

        def wview_in(w):
            return w.rearrange("(k p) f -> p k f", p=128)

        def load_expert(e, s):
            g_, e_ = divmod(e, 4)
            vg = wview_in(inp["moe_w_gate"][l, g_, e_])
            vu = wview_in(inp["moe_w_up"][l, g_, e_])
            vd = inp["moe_w_down"][l, g_, e_].rearrange("(c p) d -> p c d", p=128)
            for j in range(2):
                self.wload(wg[s][:, 4 * j:4 * j + 4, :], vg[:, 4 * j:4 * j + 4, :], (4, 512))
            for j in range(2):
                self.wload(wu[s][:, 4 * j:4 * j + 4, :], vu[:, 4 * j:4 * j + 4, :], (4, 512))
            for j in range(2):
                self.wload(wd[s][:, 2 * j:2 * j + 2, :], vd[:, 2 * j:2 * j + 2, :], (2, D))

        load_expert(0, 0)

        def bc(ap, shape):
            return ap.to_broadcast(shape)

        def routing4(c0, pl4, rt):
            S4 = [128, 4, 4]
            L = rt[:, :, 0:20]
            gm, ohg, dg, sumg, gval = rt[:, :, 20], rt[:, :, 21:25], rt[:, :, 25:29], rt[:, :, 29], rt[:, :, 30]
            es, mx1, oh1, es2, mx2, oh2 = rt[:, :, 32:36], rt[:, :, 36], rt[:, :, 37:41], rt[:, :, 41:45], rt[:, :, 45], rt[:, :, 46:50]
            dd, ed, w1, w2, win, tmp = rt[:, :, 50], rt[:, :, 51], rt[:, :, 52], rt[:, :, 53], rt[:, :, 54:58], rt[:, :, 58:62]

            def rmax(out, in_):
                K.S.add("dve", lambda e: e.tensor_reduce(out=out, in_=in_, axis=AX.X, op=ALU.max), [in_], [out])

            K.tt("dve", L, pl4, bc(b20.unsqueeze(1), [128, 4, 20]), ALU.add)
            rmax(gm, L[:, :, 0:4])
            K.tt("dve", ohg, L[:, :, 0:4], bc(gm.unsqueeze(2), S4), ALU.is_equal)
            K.tt("dve", dg, L[:, :, 0:4], bc(gm.unsqueeze(2), S4), ALU.subtract)
            K.act(dg, dg, AF.Exp)
            K.S.add("dve", lambda e: e.tensor_reduce(out=sumg, in_=dg, axis=AX.X, op=ALU.add), [dg], [sumg])
            K.recip(gval, sumg)
            K.tt("dve", es, L[:, :, 4:8], bc(ohg[:, :, 0:1], S4), ALU.mult)
            for g_ in range(1, 4):
                K.tt("dve", tmp, L[:, :, 4 + 4 * g_:8 + 4 * g_], bc(ohg[:, :, g_:g_ + 1], S4), ALU.mult)
                K.tt("dve", es, es, tmp, ALU.add)
            rmax(mx1, es)
            K.tt("dve", oh1, es, bc(mx1.unsqueeze(2), S4), ALU.is_equal)
            K.stt(es2, oh1, -1e30, es, ALU.mult, ALU.add)
            rmax(mx2, es2)
            K.tt("dve", oh2, es2, bc(mx2.unsqueeze(2), S4), ALU.is_equal)
            K.tt("dve", dd, mx2, mx1, ALU.subtract)
            K.act(ed, dd, AF.Exp)
            K.ts("dve", w1, ed, 1.0, ALU.add)
            K.recip(w1, w1)
            K.tt("dve", w2, ed, w1, ALU.mult)
            K.tt("dve", w1, w1, gval, ALU.mult)
            K.tt("dve", w2, w2, gval, ALU.mult)
            K.tt("dve", win, oh1, bc(w1.unsqueeze(2), S4), ALU.mult)
            K.tt("dve", tmp, oh2, bc(w2.unsqueeze(2), S4), ALU.mult)
            K.tt("dve", win, win, tmp, ALU.add)
            for g_ in range(4):
                K.tt("dve", cw[:, c0:c0 + 4, 4 * g_:4 * g_ + 4], win, bc(ohg[:, :, g_:g_ + 1], S4), ALU.mult)

        def prepass(c):
            ss = self.stat_slot()
            src = h[:, c, :]
            K.act(xnb, src, AF.Square, accum=ss[:, 0:1])
            K.ts("dve", ss[:, 1:2], ss[:, 0:1], 1.0 / D, ALU.mult, 1e-6, ALU.add)
            K.act(ss[:, 2:3], ss[:, 1:2], AF.Sqrt)
            K.recip(ss[:, 3:4], ss[:, 2:3])
            K.stt(xnb, src, ss[:, 3:4], self.gB, ALU.mult, ALU.mult)
            K.stt(xn32, src, ss[:, 3:4], self.gB, ALU.mult, ALU.mult)
            self.to_xnT(xnb, c, c % 2)
            p32 = K.psum[:, 6 * 512:8 * 512]
            for k in range(KC):
                K.tr(p32[:, k * 128:(k + 1) * 128], xn32[:, k * 128:(k + 1) * 128], self.ident_f)
            K.cp("act", xn32, p32)
            tbi, cc = divmod(c, 4)
            pl4 = K.bank(4 + tbi % 2)[:, 0:80]
            for k in range(KC):
                K.mm(pl4[:, cc * 20:(cc + 1) * 20], xT32[:, k, :], wr32[:, k, :], k == 0, k == KC - 1)
            if cc == 3:
                routing4(c - 3, pl4.rearrange("p (c j) -> p c j", c=4), rts[tbi % 2])

        it = 0
        io = 0
        for e in range(16):
            s = e % 2
            if e + 1 < 16:
                load_expert(e + 1, (e + 1) % 2)
            for tb in range(NTB):
                if e == 0:
                    for c in range(tb * 4, tb * 4 + 4):
                        prepass(c)
                tsl = slice(tb * 512, (tb + 1) * 512)
                hTb = hT[(e * NTB + tb) % 2]
                for fb in range(4):
                    pg = K.bank(it % 2)
                    pu = K.bank(2 + it % 2)
                    fsl = slice(fb * 128, (fb + 1) * 128)
                    for kc in range(KC):
                        K.mm(pg, wg[s][:, kc, fsl], xnT[:, kc, tsl], kc == 0, kc == KC - 1)
                    for kc in range(KC):
                        K.mm(pu, wu[s][:, kc, fsl], xnT[:, kc, tsl], kc == 0, kc == KC - 1)
                    K.act(sgt[it % 2], pg, AF.Silu)
                    K.tt("dve", hTb[:, fb, :], sgt[it % 2], pu, ALU.mult)
                    it += 1
                for cc in range(4):
                    c = tb * 4 + cc
                    for dh in range(2):
                        po = K.bank(4 + io % 4)
                        io += 1
                        dsl = slice(dh * 512, (dh + 1) * 512)
                        for fc in range(4):
                            K.mm(po, hTb[:, fc, cc * 128:(cc + 1) * 128], wd[s][:, fc, dsl], fc == 0, fc == 3)
                        K.stt(h[:, c, dsl], po, cw[:, c, e:e + 1], h[:, c, dsl], ALU.mult, ALU.add)
        A.release(m0)

    def l1mix(self):
        K, A, NCH, T = self.K, self.A, self.NCH, self.T
        assert NCH == 16
        inp = self.inp
        h, xnT = self.h, self.xnT
        m0 = A.mark()
        xnb = [A.alloc([128, D], BF16) for _ in range(2)]
        junk = A.alloc([128, D], BF16)
        self.load_gamma(inp["mix_norm"][1])
        for c in range(NCH):
            self.rms(h[:, c, :], xnb[c % 2], junk)
            self.to_xnT(xnb[c % 2], c, c % 2)
        A.release(m0)
        Wqkv = inp["c_w_qkv"][0].rearrange("(k p) n -> p k n", p=128)
        qT = A.alloc([128, 8, T], BF16)
        m1 = A.mark()
        wgrp = [A.alloc([128, KC, 512], BF16) for _ in range(2)]
        stg = [A.alloc([128, T], BF16) for _ in range(2)]
        kv_loc = [self.dint("kv_loc%d" % g, [4 * 128, T], BF16) for g in range(4)]
        kv_all = [self.dint("kv_all%d" % g, [2 * 4 * 128, T], BF16) for g in range(4)]
        it = 0
        gorder = [2, 3, 4, 5, 0, 1]

        def load_qkv_group(gi):
            g2 = gorder[gi]
            for j in range(2):
                self.wload(wgrp[gi % 2][:, 4 * j:4 * j + 4, :], Wqkv[:, 4 * j:4 * j + 4, g2 * 512:(g2 + 1) * 512], (4, 512))

        load_qkv_group(0)
        for bi in range(24):
            gi, hb = divmod(bi, 4)
            grp = gorder[gi]
            blk = grp * 4 + hb
            if hb == 0 and gi + 1 < 6:
                load_qkv_group(gi + 1)
            wb = wgrp[gi % 2][:, :, hb * 128:(hb + 1) * 128]
            for tb in range(4):
                ps = K.bank(2 + it % 4)
                it += 1
                tsl = slice(tb * 512, (tb + 1) * 512)
                for kc in range(KC):
                    K.mm(ps, wb[:, kc, :], xnT[:, kc, tsl], kc == 0, kc == KC - 1)
                if blk < 8:
                    K.act(qT[:, blk, tsl], ps, AF.Copy, scale=float(128.0 ** -0.5))
                elif tb % 2 == 0:
                    K.cp("act", stg[blk % 2][:, tsl], ps)
                else:
                    K.cp("dve", stg[blk % 2][:, tsl], ps)
            if blk >= 8:
                g4, j4 = divmod(blk - 8, 4)
                K.dma("sp", kv_loc[g4][j4 * 128:(j4 + 1) * 128, :], stg[blk % 2])
                if j4 == 3:
                    K.S.add("pool", lambda e, g4=g4: e.collective_compute(
                        "AllGather", ALU.bypass, replica_groups=self.rg,
                        ins=[kv_loc[g4].opt()], outs=[kv_all[g4].opt()]),
                        [kv_loc[g4]], [kv_all[g4]], dma="cc")
        A.release(m1)
        oT = xnT
        KTb = [A.alloc([128, 2 * T], BF16), self.stage[0].bitcast(BF16)]
        VTb = [A.alloc([128, 2 * T], BF16), self.stage[1].bitcast(BF16)]
        sqb = [A.alloc([128, 512], BF16) for _ in range(2)]
        maskOP = A.alloc([128, 2, 128], BF16)
        NR = 4
        Vd = [A.alloc([128, 128], BF16) for _ in range(NR)]
        PT = [A.alloc([128, 256], BF16) for _ in range(NR)]
        Oa = A.alloc([128, 1024], F32)
        La = A.alloc([128, 1024], F32)
        rl = A.alloc([128, 1024], F32)
        sm = A.alloc([128, 16], F32)
        cb = A.alloc([128, 2], BF16)
        cneg = A.alloc([128, 2], F32)
        K.memset("pool", maskOP, 1.0)
        K.asel(maskOP[:, 0, :], maskOP[:, 0, :], [[1, 128]], ALU.is_ge, 0.0, 0, -1)
        K.asel(maskOP[:, 1, :], maskOP[:, 1, :], [[-1, 128]], ALU.is_ge, 0.0, 0, 1)
        onec = self.ones_b[:, 0:1]

        def prologue(hd):
            KT, VT = KTb[hd % 2], VTb[hd % 2]
            gk, gv, j4 = hd // 4, 2 + hd // 4, hd % 4
            K.dma("sp", KT[:, 0:T], kv_all[gk][j4 * 128:(j4 + 1) * 128, :])
            K.dma("sp", KT[:, T:2 * T], kv_loc[gk][j4 * 128:(j4 + 1) * 128, :])
            K.dma("sp", VT[:, 0:T], kv_all[gv][j4 * 128:(j4 + 1) * 128, :])
            K.dma("sp", VT[:, T:2 * T], kv_loc[gv][j4 * 128:(j4 + 1) * 128, :])
            pn = K.bank(7)
            for j in range(12):
                sq = sqb[j % 2]
                src = KT[:, j * 512:(j + 1) * 512] if j < 8 else qT[:, hd, (j - 8) * 512:(j - 7) * 512]
                K.tt("pool", sq, src, src, ALU.mult)
                K.mm(pn[0:1, :], onec, sq)
                K.S.add("dve", lambda e, j=j: e.tensor_reduce(out=sm[0:1, j:j + 1], in_=pn[0:1, :], axis=AX.X, op=ALU.max),
                        [pn[0:1, :]], [sm[0:1, j:j + 1]])
            K.S.add("dve", lambda e: e.tensor_reduce(out=sm[0:1, 12:13], in_=sm[0:1, 0:8], axis=AX.X, op=ALU.max),
                    [sm[0:1, 0:8]], [sm[0:1, 12:13]])
            K.S.add("dve", lambda e: e.tensor_reduce(out=sm[0:1, 13:14], in_=sm[0:1, 8:12], axis=AX.X, op=ALU.max),
                    [sm[0:1, 8:12]], [sm[0:1, 13:14]])
            K.tt("dve", sm[0:1, 14:15], sm[0:1, 12:13], sm[0:1, 13:14], ALU.mult)
            K.act(cb[0:1, hd % 2:hd % 2 + 1], sm[0:1, 14:15], AF.Sqrt)
            K.mm(pn[:, 0:1], self.ones_b[0:1, :], cb[0:1, hd % 2:hd % 2 + 1])
            K.act(cneg[:, hd % 2:hd % 2 + 1], pn[:, 0:1], AF.Copy, scale=-1.0)

        units = []
        for hd in range(8):
            for qh in range(2):
                sub1 = []
                for kb in range(8 * qh - 1, 8 * qh + 8):
                    qb = [b_ for b_ in (kb, kb + 1) if 8 * qh <= b_ < 8 * qh + 8]
                    kinds = [0 if b_ == kb else 1 for b_ in qb]
                    u = dict(hd=hd, qh=qh, ks=ssl(T + 128 * kb, 128, 1), qs=ssl(128 * qb[0], 128 * len(qb), 1),
                             NQ=128 * len(qb), kinds=kinds, j0=0, flg=(kb == -1), pv=[])
                    for i_, b_ in enumerate(qb):
                        bl = b_ - 8 * qh
                        u["pv"].append([bl // 4, slice((bl % 4) * 128, (bl % 4) * 128 + 128), slice(128 * i_, 128 * i_ + 128)])
                    sub1.append(u)
                for r in range(4):
                    for kb in range(2 * qh - 1, 2 * qh + 2):
                        qb = [b_ for b_ in (kb, kb + 1) if 2 * qh <= b_ < 2 * qh + 2]
                        kinds = [0 if b_ == kb else 1 for b_ in qb]
                        u = dict(hd=hd, qh=qh, ks=ssl(T + 512 * kb + r, 128, 4), qs=ssl(512 * qb[0] + r, 128 * len(qb), 4),
                                 NQ=128 * len(qb), kinds=kinds, j0=0, flg=(kb == -1), pv=[])
                        for i_, b_ in enumerate(qb):
                            u["pv"].append([b_ - 2 * qh, ssl(r, 128, 4), slice(128 * i_, 128 * i_ + 128)])
                        sub1.append(u)
                sub2 = []
                for r in range(16):
                    for kind in (1, 0):
                        k0 = r if kind == 1 else T + r
                        u = dict(hd=hd, qh=qh, ks=ssl(k0, 128, 16), qs=ssl(16 * 64 * qh + r, 64, 16), NQ=64,
                                 kinds=[kind], j0=64 * qh, flg=(kind == 1),
                                 pv=[[r // 8, slice((r % 8) * 64, (r % 8) * 64 + 64), slice(0, 64)]])
                        sub2.append(u)
                for sub, endk in ((sub1, "evac"), (sub2, "final")):
                    first, last = {}, {}
                    for ui, u in enumerate(sub):
                        for pi, p_ in enumerate(u["pv"]):
                            first.setdefault(p_[0], (ui, pi))
                            last[p_[0]] = (ui, pi)
                    for ui, u in enumerate(sub):
                        for pi, p_ in enumerate(u["pv"]):
                            p_.append(first[p_[0]] == (ui, pi))
                            p_.append(last[p_[0]] == (ui, pi))
                        u["end"] = endk if ui == len(sub) - 1 else None
                    units.extend(sub)

        def stageA_pe(idx):
            u = units[idx]
            hd = u["hd"]
            KT, VT = KTb[hd % 2], VTb[hd % 2]
            pvt = K.bank(7, 128, BF16)
            K.tr(pvt, VT[:, u["ks"]], self.ident_b)
            ps = K.bank(4 + idx % 3)
            K.mm(ps[:, 0:u["NQ"]], KT[:, u["ks"]], qT[:, hd, u["qs"]], True, True)

        def stageA_cp(idx):
            pvt = K.bank(7, 128, BF16)
            K.cp("act" if idx % 2 == 0 else "dve", Vd[idx % NR], pvt)

        def stageB(idx):
            u = units[idx]
            hd, NQ, j0 = u["hd"], u["NQ"], u["j0"]
            ps = K.bank(4 + idx % 3)
            ptv = PT[idx % NR][:, 0:NQ]
            K.act(ptv, ps[:, 0:NQ], AF.Exp, bias=cneg[:, hd % 2:hd % 2 + 1])
            if len(u["kinds"]) == 2:
                K.tt("dve", ptv.rearrange("p (a q) -> p a q", a=2), ptv.rearrange("p (a q) -> p a q", a=2), maskOP, ALU.mult)
            else:
                mk = maskOP[:, u["kinds"][0], j0:j0 + NQ]
                if u["flg"]:
                    K.stt(ptv, ptv, self.flag, mk, ALU.mult, ALU.mult)
                else:
                    K.tt("dve", ptv, ptv, mk, ALU.mult)

        def stageC(idx):
            u = units[idx]
            hd, qh = u["hd"], u["qh"]
            vd = Vd[idx % NR]
            pt = PT[idx % NR]
            for (b, osl, psl, st_, sp_) in u["pv"]:
                K.mm(K.bank(b)[:, osl], vd, pt[:, psl], st_, sp_)
                K.mm(K.bank(2 + b)[:, osl], self.ones_b, pt[:, psl], st_, sp_)
            Oacc = K.psum[:, 0:2 * 512]
            Lacc = K.psum[:, 2 * 512:4 * 512]
            if u["end"] == "evac":
                K.cp("act", Oa, Oacc)
                K.cp("dve", La, Lacc)
            elif u["end"] == "final":
                nat = "p (r j) -> p j r"
                K.tt("dve", La.rearrange("p (j r) -> p j r", r=16), La.rearrange("p (j r) -> p j r", r=16),
                     Lacc.rearrange(nat, r=16), ALU.add)
                K.act(rl, La, AF.Ln)
                K.act(rl, rl, AF.Exp, scale=-1.0)
                K.tt("dve", Oa.rearrange("p (j r) -> p j r", r=16), Oa.rearrange("p (j r) -> p j r", r=16),
                     Oacc.rearrange(nat, r=16), ALU.add)
                K.tt("dve", oT[:, hd, qh * 1024:(qh + 1) * 1024], Oa, rl, ALU.mult)

        NU = len(units)
        prologue(0)
        prologue(1)
        stageA_pe(0)
        stageA_cp(0)
        stageA_pe(1)
        stageA_cp(1)
        stageB(0)
        for idx in range(NU):
            u = units[idx]
            if idx > 0 and units[idx - 1]["hd"] != u["hd"] and u["hd"] + 1 < 8:
                prologue(u["hd"] + 1)
            if idx + 2 < NU:
                stageA_pe(idx + 2)
            if idx + 1 < NU:
                stageB(idx + 1)
            if idx + 2 < NU:
                stageA_cp(idx + 2)
            stageC(idx)
        self.dump("oT", oT, [8, 128, T], "h p t -> p h t")
        A.release(m1)
        wo = A.alloc([128, 8, D], BF16)
        Wout = inp["c_w_out"][0].rearrange("(k p) n -> p k n", p=128)
        for cg in range(2):
            for j in range(2):
                self.wload(wo[:, 4 * j:4 * j + 4, cg * 512:(cg + 1) * 512],
                           Wout[:, 4 * j:4 * j + 4, cg * 512:(cg + 1) * 512], (4, 512))
        for c in range(NCH):
            csl = slice(c * 128, (c + 1) * 128)
            for dh in range(2):
                po = K.bank(6 + dh)
                dsl = slice(dh * 512, (dh + 1) * 512)
                for hd in range(8):
                    K.mm(po, oT[:, hd, csl], wo[:, hd, dsl], hd == 0, hd == 7)
                K.tt("dve", h[:, c, dsl], po, h[:, c, dsl], ALU.add)
        A.release(m0)


FULL_STAGES = ("l0mix", "s5", "moe0", "l1mix", "moe1", "final")


def kernel(**inputs):
    inp = {k: np.ascontiguousarray(np.asarray(v, dtype=np.float32)) for k, v in inputs.items()}
    x = inp["x"]
    B, L, _ = x.shape
    assert (B, L) == (4, 4096)
    P = Prog(NCH=16, stages=FULL_STAGES, n_cores=NCORES)
    shared = {k: v for k, v in inp.items() if k != "x"}
    maps = []
    for c in range(NCORES):
        b, half = divmod(c, 2)
        m = dict(shared)
        m["x"] = np.ascontiguousarray(x[b, half * 2048:(half + 1) * 2048])
        m["flag"] = np.full((128, 1), float(half), np.float32)
        maps.append(m)
    res = run_bass_kernel_spmd(P.nc, maps, core_ids=list(range(NCORES)))
    out = np.empty((B, L, D), np.float32)
    for c in range(NCORES):
        b, half = divmod(c, 2)
        out[b, half * 2048:(half + 1) * 2048] = np.asarray(res.results[c]["y"], dtype=np.float32)
    return out
```
